# Optimizing a Trainium2 kernel written in Bass

```python
import math
import jax, jax.numpy as jnp
from jax import lax
import numpy as np


D_MODEL = 1024
BATCH = 8
SEQ = 2048
DEPTH = 2
DEC_BATCH = 128
DEC_SEQ = 8
PAST_LEN = 16384
PAGE_SIZE = 128

N_MEM = 256
GM_WIDTH = D_MODEL
GM_CHUNK = 128
GM_GROUP = 128
GM_GROUPS = GM_WIDTH // GM_GROUP
SSD_EXPAND = 2
SSD_INNER = SSD_EXPAND * D_MODEL
SSD_HEADDIM = 64
SSD_HEADS = SSD_INNER // SSD_HEADDIM
SSD_STATE = 128
SSD_GROUPS = 4
SSD_HPG = SSD_HEADS // SSD_GROUPS
SSD_CONV = 4
SSD_CHUNK = 128
CONV_DIM = SSD_INNER + 2 * SSD_GROUPS * SSD_STATE
XA_HEADS = 4
XA_HEADDIM = D_MODEL // XA_HEADS
XA_WIDTH = XA_HEADS * XA_HEADDIM
N_BRANCH = 3
IN_DIM = 2 * GM_WIDTH + SSD_INNER + CONV_DIM + SSD_HEADS + XA_WIDTH + N_BRANCH * D_MODEL
D_FF = ((8 * D_MODEL // 3 + 127) // 128) * 128
N_EXPERTS = 8
TOP_K = 2
E_FF = 7 * D_MODEL // 2
N_DENSE = (DEPTH + 1) // 2
N_MOE = DEPTH // 2
ALPHA = (2 * DEPTH) ** 0.25
BETA = (8 * DEPTH) ** -0.25
LN_EPS = 1e-5

kernel_name = 'hybrid_gmlp_ssd_memxattn_deepnorm_step'


def layer_norm(x, g, b):
    xf = x.astype(jnp.float32)
    mu = jnp.mean(xf, -1, keepdims=True)
    var = jnp.mean(jnp.square(xf - mu), -1, keepdims=True)
    return ((xf - mu) * lax.rsqrt(var + LN_EPS)).astype(x.dtype) * g + b


def grouped_rms_norm(y, g):
    bsz, l, _ = y.shape
    yg = y.astype(jnp.float32).reshape(bsz, l, SSD_GROUPS, SSD_INNER // SSD_GROUPS)
    yg = yg * lax.rsqrt(jnp.mean(yg * yg, -1, keepdims=True) + LN_EPS)
    return yg.reshape(bsz, l, SSD_INNER).astype(y.dtype) * g


def gmlp_spatial(v, w_s, b_s):
    bsz, l, _ = v.shape
    n_chunks = -(-l // GM_CHUNK)
    pad = n_chunks * GM_CHUNK - l
    vc = jnp.pad(v, ((0, 0), (0, pad), (0, 0))).reshape(bsz, n_chunks, GM_CHUNK, GM_GROUPS, GM_GROUP)
    causal = jnp.tril(jnp.ones((GM_CHUNK, GM_CHUNK), dtype=bool))
    w = jnp.where(causal, w_s, 0.0)
    z = jnp.einsum('gij,bcjgd->bcigd', w, vc) + b_s.T[None, None, :, :, None]
    return z.reshape(bsz, n_chunks * GM_CHUNK, GM_WIDTH)[:, :l]


def causal_conv(xbc, prev, w, b):
    l = xbc.shape[1]
    full = jnp.concatenate([prev.astype(xbc.dtype), xbc], axis=1)
    out = b + full[:, 0:l] * w[0]
    for k in range(1, SSD_CONV):
        out = out + full[:, k:k + l] * w[k]
    return out, full[:, l:]


def ssd_chunked(x, dt, a, bm, cm, h0):
    f32 = jnp.float32
    bsz, l = x.shape[0], x.shape[1]
    q = min(SSD_CHUNK, l)
    nc = -(-l // q)
    pad = nc * q - l

    def padc(t):
        return jnp.pad(t.astype(f32), [(0, 0), (0, pad)] + [(0, 0)] * (t.ndim - 2))

    xp, dtp, bp, cp = padc(x), padc(dt), padc(bm), padc(cm)
    xdt = (xp * dtp[..., None]).reshape(bsz, nc, q, SSD_GROUPS, SSD_HPG, SSD_HEADDIM)
    bc = bp.reshape(bsz, nc, q, SSD_GROUPS, SSD_STATE)
    cc = cp.reshape(bsz, nc, q, SSD_GROUPS, SSD_STATE)
    adt = (dtp * a.astype(f32)).reshape(bsz, nc, q, SSD_GROUPS, SSD_HPG)
    a_cs = jnp.cumsum(jnp.moveaxis(adt, 2, -1), axis=-1)
    tril = jnp.tril(jnp.ones((q, q), dtype=bool))
    seg = a_cs[..., :, None] - a_cs[..., None, :]
    decay = jnp.exp(jnp.where(tril, seg, -jnp.inf))
    cb = jnp.einsum('bcign,bcjgn->bcgij', cc, bc)
    y_diag = jnp.einsum('bcgij,bcgrij,bcjgrp->bcigrp', cb, decay, xdt)
    to_end = jnp.exp(a_cs[..., -1:] - a_cs)
    chunk_states = jnp.einsum('bcjgn,bcgrj,bcjgrp->bcgrpn', bc, to_end, xdt)
    chunk_decay = jnp.exp(a_cs[..., -1])

    def step(h, inp):
        s_c, d_c = inp
        return d_c[..., None, None] * h + s_c, h

    h_init = h0.astype(f32).reshape(bsz, SSD_GROUPS, SSD_HPG, SSD_HEADDIM, SSD_STATE)
    h_last, h_in = lax.scan(step, h_init, (jnp.moveaxis(chunk_states, 1, 0), jnp.moveaxis(chunk_decay, 1, 0)))
    h_in = jnp.moveaxis(h_in, 0, 1)
    y_off = jnp.einsum('bcign,bcgrpn,bcgri->bcigrp', cc, h_in, jnp.exp(a_cs))
    y = (y_diag + y_off).reshape(bsz, nc * q, SSD_HEADS, SSD_HEADDIM)[:, :l]
    return y.astype(x.dtype), h_last.reshape(bsz, SSD_HEADS, SSD_HEADDIM, SSD_STATE).astype(h0.dtype)


def mixer(x, mem_k, mem_v, conv_prev, ssm_prev, w_in, conv_w, conv_b, dt_bias, a_log, d_skip,
          ssd_norm_g, v_ln_g, v_ln_b, w_s, b_s, p_gm, p_ssd, p_xa, w_out):
    bsz, l, _ = x.shape
    s1 = 2 * GM_WIDTH
    s2 = s1 + SSD_INNER
    s3 = s2 + CONV_DIM
    s4 = s3 + SSD_HEADS
    s5 = s4 + XA_WIDTH
    uv, z, xbc, dt, q, gates = jnp.split(x @ w_in, [s1, s2, s3, s4, s5], axis=-1)

    u, v = jnp.split(jax.nn.gelu(uv), 2, axis=-1)
    v = layer_norm(v, v_ln_g, v_ln_b)
    y_gm = u * gmlp_spatial(v, w_s, b_s)

    xbc_c, conv_new = causal_conv(xbc, conv_prev, conv_w, conv_b)
    xbc_c = jax.nn.silu(xbc_c)
    xs, bm, cm = jnp.split(xbc_c, [SSD_INNER, SSD_INNER + SSD_GROUPS * SSD_STATE], axis=-1)
    xs = xs.reshape(bsz, l, SSD_HEADS, SSD_HEADDIM)
    dt = jax.nn.softplus((dt + dt_bias).astype(jnp.float32))
    a = -jnp.exp(a_log.astype(jnp.float32))
    y, ssm_new = ssd_chunked(xs, dt, a, bm.reshape(bsz, l, SSD_GROUPS, SSD_STATE),
                             cm.reshape(bsz, l, SSD_GROUPS, SSD_STATE), ssm_prev)
    y = (y + xs * d_skip[:, None]).reshape(bsz, l, SSD_INNER) * jax.nn.silu(z)
    y_ssd = grouped_rms_norm(y, ssd_norm_g)

    qh = q.reshape(bsz, l, XA_HEADS, XA_HEADDIM)
    s = jnp.einsum('blhd,bmhd->bhlm', qh, mem_k).astype(jnp.float32) * (XA_HEADDIM ** -0.5)
    p = jax.nn.softmax(s, axis=-1).astype(x.dtype)
    y_xa = jnp.einsum('bhlm,bmhd->blhd', p, mem_v).reshape(bsz, l, XA_WIDTH)

    g = jax.nn.sigmoid(gates).reshape(bsz, l, N_BRANCH, D_MODEL)
    merged = g[:, :, 0] * (y_gm @ p_gm) + g[:, :, 1] * (y_ssd @ p_ssd) + g[:, :, 2] * (y_xa @ p_xa)
    return merged @ w_out, conv_new, ssm_new, v


def swiglu(x, wg, wu, wd):
    return (jax.nn.silu(x @ wg) * (x @ wu)) @ wd


def moe_swiglu(x, router_w, router_b, wg, wu, wd):
    logits = (x @ router_w).astype(jnp.float32) + router_b
    top_v, top_i = lax.top_k(logits, TOP_K)
    gate = jax.nn.softmax(top_v, axis=-1)
    out = jnp.zeros_like(x)
    for e in range(N_EXPERTS):
        w_e = jnp.sum(jnp.where(top_i == e, gate, 0.0), axis=-1).astype(x.dtype)
        out = out + w_e[..., None] * swiglu(x, wg[e], wu[e], wd[e])
    return out


def setup_inputs(seed: int = 0) -> dict:
    key = jax.random.key(seed)
    kit = iter(jax.random.split(key, 64))

    def nrm(shape, scale):
        return jax.random.normal(next(kit), shape, jnp.float32) * scale

    u_dt = jax.random.uniform(next(kit), (DEPTH, SSD_HEADS), jnp.float32)
    dt0 = jnp.exp(u_dt * (math.log(0.1) - math.log(1e-3)) + math.log(1e-3))
    dt_bias = dt0 + jnp.log(-jnp.expm1(-dt0))
    a_log = jnp.log(jax.random.uniform(next(kit), (DEPTH, SSD_HEADS), jnp.float32, minval=1.0, maxval=16.0))
    return {
        'x_prompt': nrm((BATCH, SEQ, D_MODEL), 1.0),
        'x_sample': nrm((DEC_BATCH, DEC_SEQ, D_MODEL), 1.0),
        'mem_prompt': nrm((BATCH, N_MEM, D_MODEL), 1.0),
        'cache_mem_k': nrm((DEPTH, DEC_BATCH, N_MEM, XA_HEADS, XA_HEADDIM), 1.0),
        'cache_mem_v': nrm((DEPTH, DEC_BATCH, N_MEM, XA_HEADS, XA_HEADDIM), 1.0),
        'state_conv': nrm((DEPTH, DEC_BATCH, SSD_CONV - 1, CONV_DIM), 1.0),
        'state_ssm': nrm((DEPTH, DEC_BATCH, SSD_HEADS, SSD_HEADDIM, SSD_STATE), 0.1),
        'w_in': nrm((DEPTH, D_MODEL, IN_DIM), D_MODEL ** -0.5),
        'conv_w': nrm((DEPTH, SSD_CONV, CONV_DIM), SSD_CONV ** -0.5),
        'conv_b': nrm((DEPTH, CONV_DIM), 0.01),
        'dt_bias': dt_bias,
        'a_log': a_log,
        'd_skip': 1.0 + nrm((DEPTH, SSD_HEADS), 0.01),
        'ssd_norm_g': 1.0 + nrm((DEPTH, SSD_INNER), 0.01),
        'v_ln_g': 1.0 + nrm((DEPTH, GM_WIDTH), 0.01),
        'v_ln_b': nrm((DEPTH, GM_WIDTH), 0.01),
        'w_s': nrm((DEPTH, GM_GROUPS, GM_CHUNK, GM_CHUNK), 0.5 * GM_CHUNK ** -0.5),
        'b_s': 1.0 + nrm((DEPTH, GM_GROUPS, GM_CHUNK), 0.01),
        'p_gm': nrm((DEPTH, GM_WIDTH, D_MODEL), GM_WIDTH ** -0.5),
        'p_ssd': nrm((DEPTH, SSD_INNER, D_MODEL), SSD_INNER ** -0.5),
        'p_xa': nrm((DEPTH, XA_WIDTH, D_MODEL), XA_WIDTH ** -0.5),
        'w_out': nrm((DEPTH, D_MODEL, D_MODEL), BETA * D_MODEL ** -0.5),
        'w_mem_k': nrm((DEPTH, D_MODEL, XA_WIDTH), D_MODEL ** -0.5),
        'w_mem_v': nrm((DEPTH, D_MODEL, XA_WIDTH), D_MODEL ** -0.5),
        'ln1_g': 1.0 + nrm((DEPTH, D_MODEL), 0.01),
        'ln1_b': nrm((DEPTH, D_MODEL), 0.01),
        'ln2_g': 1.0 + nrm((DEPTH, D_MODEL), 0.01),
        'ln2_b': nrm((DEPTH, D_MODEL), 0.01),
        'ffn_wg': nrm((N_DENSE, D_MODEL, D_FF), D_MODEL ** -0.5),
        'ffn_wu': nrm((N_DENSE, D_MODEL, D_FF), D_MODEL ** -0.5),
        'ffn_wd': nrm((N_DENSE, D_FF, D_MODEL), BETA * D_FF ** -0.5),
        'router_w': nrm((N_MOE, D_MODEL, N_EXPERTS), D_MODEL ** -0.5),
        'router_b': nrm((N_MOE, N_EXPERTS), 0.01),
        'moe_wg': nrm((N_MOE, N_EXPERTS, D_MODEL, E_FF), D_MODEL ** -0.5),
        'moe_wu': nrm((N_MOE, N_EXPERTS, D_MODEL, E_FF), D_MODEL ** -0.5),
        'moe_wd': nrm((N_MOE, N_EXPERTS, E_FF, D_MODEL), BETA * E_FF ** -0.5),
    }


def reference(x_prompt, x_sample, mem_prompt, cache_mem_k, cache_mem_v, state_conv, state_ssm,
              w_in, conv_w, conv_b, dt_bias, a_log, d_skip, ssd_norm_g, v_ln_g, v_ln_b, w_s, b_s,
              p_gm, p_ssd, p_xa, w_out, w_mem_k, w_mem_v, ln1_g, ln1_b, ln2_g, ln2_b,
              ffn_wg, ffn_wu, ffn_wd, router_w, router_b, moe_wg, moe_wu, moe_wd):

    def run_group(x, mem_ks, mem_vs, conv_prevs, ssm_prevs, keep_v):
        conv_out, ssm_out, v_out = [], [], []
        for i in range(DEPTH):
            h, c_new, s_new, v_rows = mixer(
                x, mem_ks[i], mem_vs[i], conv_prevs[i], ssm_prevs[i], w_in[i], conv_w[i], conv_b[i],
                dt_bias[i], a_log[i], d_skip[i], ssd_norm_g[i], v_ln_g[i], v_ln_b[i], w_s[i], b_s[i],
                p_gm[i], p_ssd[i], p_xa[i], w_out[i])
            x = layer_norm(ALPHA * x + h, ln1_g[i], ln1_b[i])
            j = i // 2
            if i % 2 == 0:
                f = swiglu(x, ffn_wg[j], ffn_wu[j], ffn_wd[j])
            else:
                f = moe_swiglu(x, router_w[j], router_b[j], moe_wg[j], moe_wu[j], moe_wd[j])
            x = layer_norm(ALPHA * x + f, ln2_g[i], ln2_b[i])
            conv_out.append(c_new)
            ssm_out.append(s_new)
            if keep_v:
                v_out.append(v_rows)
        v_stack = jnp.stack(v_out) if keep_v else None
        return x, jnp.stack(conv_out), jnp.stack(ssm_out), v_stack

    bp = x_prompt.shape[0]
    mem_k_prompt = jnp.einsum('bmd,ide->ibme', mem_prompt, w_mem_k).reshape(DEPTH, bp, N_MEM, XA_HEADS, XA_HEADDIM)
    mem_v_prompt = jnp.einsum('bmd,ide->ibme', mem_prompt, w_mem_v).reshape(DEPTH, bp, N_MEM, XA_HEADS, XA_HEADDIM)
    conv0 = jnp.zeros((DEPTH, bp, SSD_CONV - 1, CONV_DIM), x_prompt.dtype)
    ssm0 = jnp.zeros((DEPTH, bp, SSD_HEADS, SSD_HEADDIM, SSD_STATE), x_prompt.dtype)

    y_prompt, conv_prompt, ssm_prompt, _ = run_group(x_prompt, mem_k_prompt, mem_v_prompt, conv0, ssm0, False)
    y_sample, conv_sample, ssm_sample, gmlp_v_sample = run_group(
        x_sample, cache_mem_k, cache_mem_v, state_conv, state_ssm, True)
    return (y_prompt, y_sample, mem_k_prompt, mem_v_prompt, conv_prompt, ssm_prompt, conv_sample, ssm_sample, gmlp_v_sample)
```

```python
from contextlib import ExitStack
import types
import numpy as np
import concourse.bass as bass
import concourse.mybir as mybir
from concourse.bass_utils import run_bass_kernel_spmd

F32 = mybir.dt.float32
BF16 = mybir.dt.bfloat16
AF = mybir.ActivationFunctionType
ALU = mybir.AluOpType

PE, ACT, DVE, POOLX, SP = "pe", "act", "dve", "pool", "sp"
POOL = DVE
ENGS = (PE, ACT, DVE, POOLX, SP)

NCORES = 8
D = 1024
DEPTH = 2
NT = 17
NTOK = NT * 128
IN_DIM = 11296
C_U, C_V, C_Z, C_XBC, C_DT, C_Q, C_G = 0, 1024, 2048, 4096, 7168, 7200, 8224
D_FF = 2816
E_FF = 3584
NEXP = 8
ALPHA = float((2 * DEPTH) ** 0.25)
EPS = 1e-5
GELU_C = 0.7978845608028654


class Buf:
    __slots__ = ("w", "r", "excl")

    def __init__(self, excl=False):
        self.w = None
        self.r = []
        self.excl = excl


class Op:
    __slots__ = ("eng", "fn", "deps", "dma_key", "ms", "dma_val", "ndep")

    def __init__(self, eng, fn, dma_key):
        self.eng = eng
        self.fn = fn
        self.deps = set()
        self.dma_key = dma_key
        self.ms = None
        self.dma_val = None
        self.ndep = 0


def _freeze(fn):
    if fn.__closure__ is None:
        return fn
    cells = []
    for c in fn.__closure__:
        try:
            cells.append(types.CellType(c.cell_contents))
        except ValueError:
            cells.append(c)
    return types.FunctionType(fn.__code__, fn.__globals__, fn.__name__, fn.__defaults__, tuple(cells))


class Sched:
    SEM_WRAP = 30000

    def __init__(self, nc):
        self.nc = nc
        self.dry = False
        self.ops = {e: [] for e in ENGS}
        self.all = []
        self.last_dma = {}

    def op(self, eng, fn, reads=(), writes=(), dma_key=None):
        if self.dry:
            return None
        o = Op(eng, _freeze(fn), dma_key)
        deps = o.deps
        if dma_key is not None:
            prev = self.last_dma.get(dma_key)
            if prev is not None:
                deps.add(prev)
            self.last_dma[dma_key] = o
        for b in reads:
            if b.w is not None:
                deps.add(b.w)
            if b.excl:
                for r_ in b.r:
                    if r_.eng != eng:
                        deps.add(r_)
        for b in writes:
            if b.w is not None:
                deps.add(b.w)
            deps.update(b.r)
        deps.discard(o)
        for b in reads:
            b.r.append(o)
        for b in writes:
            b.w = o
            b.r = []
        self.ops[eng].append(o)
        self.all.append(o)
        return o

    def finalize(self, stack):
        nc = self.nc
        for o in self.all:
            for d in o.deps:
                if d.dma_key is None and d.eng == PE and o.eng == PE and o.dma_key is None:
                    continue
                d.ndep += 1
        eng_sems = {e: [] for e in ENGS}
        for e in ENGS:
            n = 0
            for o in self.ops[e]:
                if o.dma_key is None and o.ndep > 0:
                    o.ms = n
                    n += 1
            for i in range(max(1, -(-n // self.SEM_WRAP))):
                eng_sems[e].append(stack.enter_context(nc.semaphore(f"c_{e}_{i}")))
        dma_sems, dma_cnt = {}, {}
        for o in self.all:
            if o.dma_key is not None:
                k = o.dma_key
                if k not in dma_sems:
                    dma_sems[k] = stack.enter_context(nc.semaphore(f"d_{k}"))
                    dma_cnt[k] = 0
                dma_cnt[k] += 16
                o.dma_val = dma_cnt[k]
        W = self.SEM_WRAP
        self.stats = {'ms': {e: sum(1 for o in self.ops[e] if o.ms is not None) for e in ENGS}, 'dma': dict(dma_cnt)}

        def target(d):
            if d.dma_key is not None:
                return dma_sems[d.dma_key], d.dma_val
            i, v = divmod(d.ms, W)
            return eng_sems[d.eng][i], v + 1

        block = stack.enter_context(nc.Block())
        final_waits = [(dma_sems[k], dma_cnt[k]) for k in dma_sems]
        sched = self

        def emit_engine(e, eobj):
            waited = {}
            for o in sched.ops[e]:
                need = {}
                for d in o.deps:
                    if d.dma_key is None and d.eng == PE and e == PE and o.dma_key is None:
                        continue
                    s, v = target(d)
                    key = id(s)
                    if waited.get(key, 0) >= v:
                        continue
                    if key not in need or need[key][1] < v:
                        need[key] = (s, v)
                for key, (s, v) in need.items():
                    eobj.wait_ge(s, v)
                    waited[key] = v
                ins = o.fn(eobj)
                if o.dma_key is not None:
                    ins.then_inc(dma_sems[o.dma_key], 16)
                elif o.ms is not None:
                    ins.then_inc(eng_sems[e][o.ms // W], 1)
            if e == SP:
                for s, v in final_waits:
                    eobj.wait_ge(s, v)

        @block.tensor
        def _(t):
            emit_engine(PE, t)

        @block.scalar
        def _(t):
            emit_engine(ACT, t)

        @block.vector
        def _(t):
            emit_engine(DVE, t)

        @block.gpsimd
        def _(t):
            emit_engine(POOLX, t)

        @block.sync
        def _(t):
            emit_engine(SP, t)


class TV:
    SEG = 256

    def __init__(self, arena, off, n, dt):
        self.arena, self.off, self.n, self.dt = arena, off, n, dt
        self.esz = 4 if dt == F32 else 2
        base = arena.t[:, off // 4:(off + n * self.esz + 3) // 4]
        self.ap = base if dt == F32 else base.bitcast(dt)[:, 0:n]

    def b(self, lo=0, hi=None):
        hi = self.n if hi is None else hi
        s0 = (self.off + lo * self.esz) // self.SEG
        s1 = (self.off + hi * self.esz - 1) // self.SEG
        return self.arena.segs[s0:s1 + 1]

    def v(self, pat=None, **kw):
        return self.ap if pat is None else self.ap.rearrange(pat, **kw)


class Arena:
    def __init__(self, t, nbytes):
        self.t = t
        self.nbytes = nbytes
        self.segs = [Buf() for _ in range(-(-nbytes // TV.SEG))]
        self.top = 0
        self.hi = 0

    def alloc(self, n, dt, align=256):
        esz = 4 if dt == F32 else 2
        off = -(-self.top // align) * align
        self.top = off + n * esz
        self.hi = max(self.hi, self.top)
        assert self.top <= self.nbytes, f"arena overflow {self.top} > {self.nbytes}"
        return TV(self, off, n, dt)

    def mark(self):
        return self.top

    def release(self, m):
        self.top = m


def build_program():
    nc = bass.Bass("TRN2", target_bir_lowering=False)

    def din(name, shape):
        return nc.dram_tensor(name, list(shape), F32, kind="ExternalInput").ap()

    def dout(name, shape):
        return nc.dram_tensor(name, list(shape), F32, kind="ExternalOutput").ap()

    xin = din("xin", [NTOK, D])
    mem = din("mem", [256, D])
    ck = din("ck", [DEPTH, 16, 256, D])
    cv = din("cv", [DEPTH, 16, 256, D])
    sconv = din("sconv", [DEPTH, 48, 3072])
    sssm = din("sssm", [DEPTH, 16, 32, 64, 128])
    w_in = din("w_in", [DEPTH, D, IN_DIM])
    conv_w = din("conv_w", [DEPTH, 4, 3072])
    conv_b = din("conv_b", [DEPTH, 1, 3072])
    dt_bias = din("dt_bias", [DEPTH, 32])
    a_log = din("a_log", [DEPTH, 32])
    d_skip = din("d_skip", [DEPTH, 32])
    ssd_norm_g = din("ssd_norm_g", [DEPTH, 1, 2048])
    v_ln_g = din("v_ln_g", [DEPTH, D])
    v_ln_b = din("v_ln_b", [DEPTH, D])
    w_s = din("w_s", [DEPTH, 8, 128, 128])
    b_s = din("b_s", [DEPTH, 1, 1024])
    p_gm = din("p_gm", [DEPTH, D, D])
    p_ssd = din("p_ssd", [DEPTH, 2048, D])
    p_xa = din("p_xa", [DEPTH, D, D])
    w_out = din("w_out", [DEPTH, D, D])
    w_mem_k = din("w_mem_k", [DEPTH, D, D])
    w_mem_v = din("w_mem_v", [DEPTH, D, D])
    ln1_g = din("ln1_g", [DEPTH, D])
    ln1_b = din("ln1_b", [DEPTH, D])
    ln2_g = din("ln2_g", [DEPTH, D])
    ln2_b = din("ln2_b", [DEPTH, D])
    ffn_wg = din("ffn_wg", [1, D, D_FF])
    ffn_wu = din("ffn_wu", [1, D, D_FF])
    ffn_wd = din("ffn_wd", [1, D_FF, D])
    router_w = din("router_w", [1, D, NEXP])
    router_b = din("router_b", [1, NEXP])
    moe_wg = din("moe_wg", [1, NEXP, D, E_FF])
    moe_wu = din("moe_wu", [1, NEXP, D, E_FF])
    moe_wd = din("moe_wd", [1, NEXP, E_FF, D])

    o_y = dout("o_y", [NTOK, D])
    o_memk = dout("o_memk", [DEPTH, 256, D])
    o_memv = dout("o_memv", [DEPTH, 256, D])
    o_convp = dout("o_convp", [DEPTH, 3, 3072])
    o_ssmp = dout("o_ssmp", [DEPTH, 32, 64, 128])
    o_convs = dout("o_convs", [DEPTH, 48, 3072])
    o_ssms = dout("o_ssms", [DEPTH, 16, 32, 64, 128])
    o_gv = dout("o_gv", [DEPTH, 128, D])
    scr = [nc.dram_tensor(f"scr{i}", [NTOK, D], F32).ap() for i in range(3)]

    ARENA_BYTES = 152 * 1024
    with ExitStack() as st:
        cst_t = st.enter_context(nc.sbuf_tensor("cst", [128, 15872 // 4], F32))
        ring_t = st.enter_context(nc.sbuf_tensor("ring", [128, 5 * 8192 // 4], F32))
        arena_t = st.enter_context(nc.sbuf_tensor("arena", [128, ARENA_BYTES // 4], F32))
        psum_t = [st.enter_context(nc.psum_tensor(f"ps{i}", [128, 512], F32)) for i in range(8)]

        S = Sched(nc)
        CA = Arena(cst_t, 15872)
        RA = Arena(ring_t, 5 * 8192)
        AR = Arena(arena_t, ARENA_BYTES)

        class Bank:
            def __init__(self, t):
                self.f = t[:]
                self.h = t[:].bitcast(BF16)
                self.buf = [Buf(excl=True)]

        banks = [Bank(t) for t in psum_t]
        pstate = {"i": 0}

        def PS():
            b = banks[pstate["i"] % 8]
            pstate["i"] += 1
            return b

        NSLOT = 3
        ring_slots = [RA.alloc(4096, BF16) for _ in range(NSLOT)]
        stg_slots = [RA.alloc(2048, F32) for _ in range(2)]
        stgc = {"i": 0}

        def stage_cast(dst_ap, dst_bufs, src_ap, shape):
            k = stgc["i"] % 2
            stgc["i"] += 1
            sl = stg_slots[k]
            a_, b_ = shape[1], shape[2]
            sv = sl.ap[:, 0:b_] if a_ == 1 else sl.ap[:, 0:a_ * b_].rearrange("p (a b) -> p a b", b=b_)
            S.op(SP, lambda e: e.dma_start(out=sv, in_=src_ap), writes=sl.b(), dma_key=f"stg{k}")
            S.op(ACT, lambda e: e.copy(dst_ap, sv), reads=sl.b(), writes=dst_bufs)

        ring = {"n": 0, "plan": [], "emitted": 0}
        RDEPTH = 1

        def ring_emit(j):
            s_ap, s_kc, s_n = ring["plan"][j]
            sl = ring_slots[j % NSLOT]
            dst = sl.ap[:, 0:s_kc * s_n].rearrange("p (k n) -> p k n", n=s_n)
            if s_kc == 1:
                stage_cast(sl.ap[:, 0:s_n], sl.b(0, s_n), s_ap[:, 0, :], [128, 1, s_n])
            elif s_kc * s_n <= 2048:
                stage_cast(dst, sl.b(0, s_kc * s_n), s_ap, [128, s_kc, s_n])
            else:
                h = s_kc // 2
                stage_cast(dst[:, 0:h, :], sl.b(0, h * s_n), s_ap[:, 0:h, :], [128, h, s_n])
                stage_cast(dst[:, h:s_kc, :], sl.b(h * s_n, s_kc * s_n), s_ap[:, h:s_kc, :], [128, s_kc - h, s_n])

        def wget(src, kc, n):
            i = ring["n"]
            ring["n"] += 1
            slot = ring_slots[i % NSLOT]
            view = slot.ap[:, 0:kc * n].rearrange("p (k n) -> p k n", n=n)
            if S.dry:
                ring["plan"].append((src, kc, n))
                return view, slot.b(0, kc * n)
            plan = ring["plan"]
            while ring["emitted"] <= min(i + RDEPTH, len(plan) - 1):
                ring_emit(ring["emitted"])
                ring["emitted"] += 1
            return view, slot.b(0, kc * n)

        def wv(w2d, r0, kc, c0, n):
            return w2d[r0:r0 + kc * 128, c0:c0 + n].rearrange("(k p) n -> p k n", p=128)

        def mm(bank_ap, pairs, reads, bank):
            n = len(pairs)
            for i, (l, r) in enumerate(pairs):
                S.op(PE, lambda e, l=l, r=r, i=i: e.matmul(bank_ap, l, r, start=(i == 0), stop=(i == n - 1)),
                     reads=reads, writes=bank.buf)

        ldp = {"i": 0}

        def load(eng, dst_tv, dst_ap, src, key):
            if key == "ldp":
                key = f"ldp{ldp['i'] % 8}"
                ldp["i"] += 1
            S.op(eng, lambda e: e.dma_start(out=dst_ap, in_=src), writes=dst_tv, dma_key=key)

        ident = CA.alloc(128, F32)
        identb = CA.alloc(128, BF16)
        maskU = CA.alloc(128, F32)
        Lst = CA.alloc(128, F32)
        ones = CA.alloc(128, F32)
        onesb = CA.alloc(128, BF16)
        BD = CA.alloc(128, F32)
        UBD = CA.alloc(128, F32)
        LBD = CA.alloc(128, F32)
        eighth = CA.alloc(128, F32)
        G16 = CA.alloc(128, F32)
        Sel8 = CA.alloc(128, F32)
        colmask = CA.alloc(16 * 128, BF16)
        onehot = CA.alloc(16, F32)
        mhalf = CA.alloc(1, F32)
        ones1b = CA.alloc(128, BF16)
        memT = CA.alloc(8 * 256, BF16)

        def aff(tv, ap, pattern, cmp, fill, base, cm):
            S.op(POOLX, lambda e: e.affine_select(out=ap, in_=ap, pattern=pattern, compare_op=cmp, fill=fill,
                                                 base=base, channel_multiplier=cm), reads=tv.b(), writes=tv.b())

        def memset(tv, val, eng=DVE):
            S.op(eng, lambda e: e.memset(tv.ap, val), writes=tv.b())

        def emit_consts():
            memset(ident, 0.0)
            aff(ident, ident.ap, [[-1, 128]], ALU.not_equal, 1.0, 0, 1)
            S.op(DVE, lambda e: e.tensor_copy(identb.ap, ident.ap), reads=ident.b(), writes=identb.b())
            memset(maskU, 1.0)
            aff(maskU, maskU.ap, [[1, 128]], ALU.is_ge, 0.0, 0, -1)
            memset(Lst, 1.0)
            aff(Lst, Lst.ap, [[-1, 128]], ALU.is_gt, 0.0, 0, 1)
            memset(ones, 1.0)
            memset(onesb, 1.0)
            memset(ones1b, 1.0)
            memset(eighth, 0.125)
            memset(mhalf, -0.5)
            memset(G16, 1.0)
            g3 = G16.ap[0:16, :].rearrange("p (b i) -> p b i", i=8)
            S.op(POOLX, lambda e: e.affine_select(out=g3, in_=g3, pattern=[[1, 16], [0, 8]], compare_op=ALU.is_equal,
                                                 fill=0.0, base=0, channel_multiplier=-1), reads=G16.b(), writes=G16.b())
            memset(Sel8, 1.0)
            s3 = Sel8.ap[0:8, :].rearrange("p (b i) -> p b i", i=8)
            S.op(POOLX, lambda e: e.affine_select(out=s3, in_=s3, pattern=[[0, 16], [1, 8]], compare_op=ALU.is_equal,
                                                 fill=0.0, base=0, channel_multiplier=-1), reads=Sel8.b(), writes=Sel8.b())
            bk = PS()
            mm(bk.f[:, 0:128], [(G16.ap[0:16, :], G16.ap[0:16, :])], G16.b(), bk)
            S.op(DVE, lambda e: e.tensor_copy(BD.ap, bk.f[:, 0:128]), reads=bk.buf, writes=BD.b())
            S.op(DVE, lambda e: e.tensor_tensor(UBD.ap, BD.ap, maskU.ap, ALU.mult), reads=BD.b() + maskU.b(), writes=UBD.b())
            S.op(DVE, lambda e: e.tensor_tensor(LBD.ap, BD.ap, Lst.ap, ALU.mult), reads=BD.b() + Lst.b(), writes=LBD.b())
            bk2 = PS()
            S.op(PE, lambda e: e.transpose(bk2.f[:, 0:16], G16.ap[0:16, :], ident.ap[0:16, 0:16]), reads=G16.b() + ident.b(), writes=bk2.buf)
            S.op(DVE, lambda e: e.tensor_copy(onehot.ap, bk2.f[:, 0:16]), reads=bk2.buf, writes=onehot.b())
            for hb in range(4):
                bk3 = PS()
                R = AR.alloc(4 * 128, F32)
                r3 = R.ap[0:16, :].rearrange("p (b i) -> p b i", i=128)
                S.op(DVE, lambda e, r3=r3: e.tensor_copy(r3, G16.ap[0:16, :].unsqueeze(1).to_broadcast([16, 4, 128])), reads=G16.b(), writes=R.b())
                S.op(POOLX, lambda e, r3=r3, hb=hb: e.affine_select(out=r3, in_=r3, pattern=[[1, 4], [0, 128]], compare_op=ALU.is_equal,
                                                                  fill=0.0, base=hb * 4, channel_multiplier=-1), reads=R.b(), writes=R.b())
                mm(bk3.f, [(ones.ap[0:16, :], R.ap[0:16, :])], ones.b() + R.b(), bk3)
                S.op(DVE, lambda e, hb=hb, bk3=bk3: e.tensor_copy(colmask.ap[:, hb * 512:(hb + 1) * 512], bk3.f), reads=bk3.buf, writes=colmask.b())
            mtmp = AR.alloc(2 * 1024, F32)
            load(SP, mtmp.b(), mtmp.v("p (a d) -> p a d", a=2), mem.rearrange("(a p) d -> p a d", p=128), "ldx")
            for a in range(2):
                for k4 in range(2):
                    bk4 = PS()
                    for kk in range(4):
                        k = k4 * 4 + kk
                        S.op(PE, lambda e, a=a, k=k, kk=kk, bk4=bk4: e.transpose(bk4.f[:, kk * 128:(kk + 1) * 128], mtmp.ap[:, a * 1024 + k * 128: a * 1024 + (k + 1) * 128], ident.ap),
                             reads=mtmp.b() + ident.b(), writes=bk4.buf)
                    dst = memT.v("p (k m) -> p k m", m=256)[:, k4 * 4:(k4 + 1) * 4, a * 128:(a + 1) * 128]
                    S.op(ACT, lambda e, dst=dst, bk4=bk4: e.copy(dst, bk4.f.rearrange("p (k m) -> p k m", m=128)), reads=bk4.buf, writes=memT.b())

        def layer_norm(x_tv, x_ap, g_tv, b_tv, tmp, eps, out_ap=None, out_tv=None, out2=None):
            stats = tmp.ap[:, 0:12].rearrange("p (c f) -> p c f", f=6)
            mv = tmp.ap[:, 12:14]
            rstd = tmp.ap[:, 14:15]
            xv = x_ap.rearrange("p (c f) -> p c f", f=512)
            for c in range(2):
                S.op(DVE, lambda e, c=c: e.bn_stats(stats[:, c, :], xv[:, c, :]), reads=x_tv, writes=tmp.b())
            S.op(DVE, lambda e: e.bn_aggr(mv, stats), reads=tmp.b(), writes=tmp.b())
            S.op(DVE, lambda e: e.tensor_scalar_add(mv[:, 1:2], mv[:, 1:2], eps), reads=tmp.b(), writes=tmp.b())
            S.op(POOLX, lambda e: e.tensor_tensor(rstd, mv[:, 1:2], mhalf.ap, ALU.pow), reads=tmp.b() + mhalf.b(), writes=tmp.b())
            S.op(DVE, lambda e: e.tensor_scalar(x_ap, x_ap, mv[:, 0:1], rstd, ALU.subtract, ALU.mult), reads=x_tv + tmp.b(), writes=x_tv)
            S.op(POOL, lambda e: e.tensor_tensor(x_ap, x_ap, g_tv.ap, ALU.mult), reads=x_tv + g_tv.b(), writes=x_tv)
            o_ap = x_ap if out_ap is None else out_ap
            o_tv = x_tv if out_tv is None else out_tv
            S.op(POOL, lambda e: e.tensor_tensor(o_ap, x_ap, b_tv.ap, ALU.add), reads=x_tv + b_tv.b(), writes=o_tv)
            if out2 is not None:
                S.op(POOL, lambda e: e.tensor_tensor(out2[1], x_ap, b_tv.ap, ALU.add), reads=x_tv + b_tv.b(), writes=out2[0])

        def rows_to_cols(rows_tv, rows_ap, nrows, nchunks, dst_tv, dst_ap3, scale=None):
            per = 512 // nrows
            c = 0
            while c < nchunks:
                n = min(per, nchunks - c)
                bk = PS()
                for i in range(n):
                    S.op(PE, lambda e, i=i, c=c, bk=bk: e.transpose(bk.f[:, i * nrows:(i + 1) * nrows], rows_ap[0:nrows, (c + i) * 128:(c + i + 1) * 128], ident.ap[0:nrows, 0:nrows]),
                         reads=rows_tv + ident.b(), writes=bk.buf)
                src = bk.f[:, 0:n * nrows].rearrange("p (c r) -> p c r", r=nrows)
                if nrows == 1:
                    src = bk.f[:, 0:n]
                    dst2 = dst_ap3[:, c:c + n]
                    S.op(DVE, lambda e, src=src, dst2=dst2: e.tensor_copy(dst2, src), reads=bk.buf, writes=dst_tv)
                elif scale is None:
                    S.op(DVE, lambda e, c=c, n=n, src=src: e.tensor_copy(dst_ap3[:, c:c + n, :], src), reads=bk.buf, writes=dst_tv)
                else:
                    S.op(DVE, lambda e, c=c, n=n, src=src: e.tensor_scalar_mul(dst_ap3[:, c:c + n, :], src, scale), reads=bk.buf, writes=dst_tv)
                c += n

        def cols_to_rows(src_tv, src_ap3, nrows, nchunks, rows_tv, rows_ap):
            c = 0
            while c < nchunks:
                n = min(4, nchunks - c)
                bk = PS()
                for i in range(n):
                    S.op(PE, lambda e, i=i, c=c, bk=bk: e.transpose(bk.f[0:nrows, i * 128:(i + 1) * 128], src_ap3[:, c + i, :], ident.ap),
                         reads=src_tv + ident.b(), writes=bk.buf)
                S.op(ACT, lambda e, c=c, n=n, bk=bk: e.copy(rows_ap[0:nrows, c * 128:(c + n) * 128], bk.f[0:nrows, 0:n * 128]), reads=bk.buf, writes=rows_tv)
                c += n

        import os as _os
        STOP = _os.environ.get("MK_STOP", "")

        class _Stop(Exception):
            pass

        ckc = {}

        def ckpt(name):
            ckc[name] = ckc.get(name, 0) + 1
            if STOP == name or STOP == f"{name}#{ckc[name]}":
                raise _Stop()

        def program():
            ckc.clear()
            try:
                program_()
            except _Stop:
                pass

        def program_():
            pstate["i"] = 0
            ring["n"] = 0
            AR.top = 0
            emit_consts()
            ckpt('consts')
            AR.top = 0
            for l in range(DEPTH):
                x_src = xin if l == 0 else scr[2]
                x_mid = scr[l % 2]
                x_dst = o_y if l == DEPTH - 1 else scr[2]
                mixer_layer(l, x_src, x_mid)
                ckpt(f'mixer{l}')
                AR.top = 0
                ffn_layer(l, x_mid, x_dst)
                ckpt(f'ffn{l}')
                AR.top = 0

        def mixer_layer(l, x_src, x_mid):
            W = w_in[l]
            vg = AR.alloc(1024, F32); vb = AR.alloc(1024, F32)
            l1g, l1b = vg, vb
            dtb = AR.alloc(32, F32); aneg = AR.alloc(32, F32); dsk = AR.alloc(32, F32)
            for tv, src in ((dtb, dt_bias), (aneg, a_log), (dsk, d_skip)):
                load(SP, tv.b(), tv.ap, src[l:l + 1, :].partition_broadcast(128), "ldp")
            S.op(ACT, lambda e: e.activation(aneg.ap, aneg.ap, AF.Exp), reads=aneg.b(), writes=aneg.b())
            S.op(DVE, lambda e: e.tensor_scalar_mul(aneg.ap, aneg.ap, -1.0), reads=aneg.b(), writes=aneg.b())
            wdt = AR.alloc(8 * 32, BF16)
            stage_cast(wdt.v("p (k n) -> p k n", n=32), wdt.b(), wv(W, 0, 8, C_DT, 32), [128, 8, 32])
            ckpt('p1')
            cw = AR.alloc(24 * 5, F32)
            gss = AR.alloc(16, F32)
            m0 = AR.mark()
            rows = AR.alloc(3072, F32)
            load(SP, rows.b(), rows.ap[0:4, :], conv_w[l], "ldp")
            load(SP, rows.b(), rows.ap[4:5, :], conv_b[l], "ldp")
            rows_to_cols(rows.b(), rows.ap, 5, 24, cw.b(), cw.v("p (c r) -> p c r", r=5), scale=0.5)
            ckpt('p1a')
            rows2 = AR.alloc(2048, F32)
            load(SP, rows2.b(), rows2.ap[0:1, :], ssd_norm_g[l], "ldp")
            rows_to_cols(rows2.b(), rows2.ap, 1, 16, gss.b(), gss.ap)
            AR.release(m0)
            ckpt('p2')
            WmT = AR.alloc(8 * 128, BF16)
            WsT = AR.alloc(8 * 128, BF16)
            bsh = AR.alloc(1024, BF16); bsl = AR.alloc(1024, BF16)
            bshs = AR.alloc(1024, BF16); bsls = AR.alloc(1024, BF16)
            m0 = AR.mark()
            wsf = AR.alloc(8 * 128, F32)
            load(SP, wsf.b(), wsf.v("p (g j) -> p g j", j=128), w_s[l].rearrange("g i j -> i g j"), "ldp")
            wmf = AR.alloc(8 * 128, F32)
            for g4 in range(2):
                bk = PS()
                for gg in range(4):
                    g = g4 * 4 + gg
                    S.op(PE, lambda e, g=g, gg=gg, bk=bk: e.transpose(bk.f[:, gg * 128:(gg + 1) * 128], wsf.ap[:, g * 128:(g + 1) * 128], ident.ap),
                         reads=wsf.b() + ident.b(), writes=bk.buf)
                src = bk.f.rearrange("p (g i) -> p g i", i=128)
                mk = maskU.ap.unsqueeze(1).to_broadcast([128, 4, 128])
                dstf = wmf.v("p (g i) -> p g i", i=128)[:, g4 * 4:(g4 + 1) * 4, :]
                dstb = WmT.v("p (g i) -> p g i", i=128)[:, g4 * 4:(g4 + 1) * 4, :]
                S.op(DVE, lambda e, dstf=dstf, src=src, mk=mk: e.tensor_tensor(dstf, src, mk, ALU.mult), reads=bk.buf + maskU.b(), writes=wmf.b())
                S.op(DVE, lambda e, dstf=dstf, dstb=dstb: e.tensor_copy(dstb, dstf), reads=wmf.b(), writes=WmT.b())
            xrep = AR.alloc(8 * 128, F32)
            x4 = xrep.ap[0:8, :].rearrange("p (g b i) -> p g b i", b=16, i=8)
            S.op(DVE, lambda e: e.tensor_copy(x4, wmf.v("p (g i) -> p g i", i=128)[0:8, :, 0:8].unsqueeze(2).to_broadcast([8, 8, 16, 8])), reads=wmf.b(), writes=xrep.b())
            for g4 in range(2):
                bk = PS()
                for gg in range(4):
                    g = g4 * 4 + gg
                    mm(bk.f[:, gg * 128:(gg + 1) * 128], [(Sel8.ap[0:8, :], xrep.ap[0:8, g * 128:(g + 1) * 128])], Sel8.b() + xrep.b(), bk)
                src = bk.f.rearrange("p (g i) -> p g i", i=128)
                mk = BD.ap.unsqueeze(1).to_broadcast([128, 4, 128])
                dstb = WsT.v("p (g i) -> p g i", i=128)[:, g4 * 4:(g4 + 1) * 4, :]
                S.op(DVE, lambda e, dstb=dstb, src=src, mk=mk: e.tensor_tensor(dstb, src, mk, ALU.mult), reads=bk.buf + BD.b(), writes=WsT.b())
            ckpt('p3')
            bsf = AR.alloc(1024, F32); bst = AR.alloc(1024, F32)
            load(SP, bsf.b(), bsf.ap[0:1, :], b_s[l], "ldp")
            S.op(DVE, lambda e: e.tensor_copy(bsh.ap[0:1, :], bsf.ap[0:1, :]), reads=bsf.b(), writes=bsh.b())
            S.op(DVE, lambda e: e.tensor_copy(bst.ap[0:1, :], bsh.ap[0:1, :]), reads=bsh.b(), writes=bst.b())
            S.op(DVE, lambda e: e.tensor_sub(bst.ap[0:1, :], bsf.ap[0:1, :], bst.ap[0:1, :]), reads=bsf.b() + bst.b(), writes=bst.b())
            S.op(DVE, lambda e: e.tensor_copy(bsl.ap[0:1, :], bst.ap[0:1, :]), reads=bst.b(), writes=bsl.b())
            for srcv, dstv in ((bsh, bshs), (bsl, bsls)):
                s_ = srcv.ap[0:1, :].rearrange("p (g i) -> p g i", i=128)[:, :, 0:8].unsqueeze(2).to_broadcast([1, 8, 16, 8])
                d_ = dstv.ap[0:1, :].rearrange("p (g b i) -> p g b i", b=16, i=8)
                S.op(DVE, lambda e, s_=s_, d_=d_: e.tensor_copy(d_, s_), reads=srcv.b(), writes=dstv.b())
            AR.release(m0)

            ckpt('p4')
            kT = AR.alloc(8 * 256, BF16)
            vtk = AR.alloc(2 * 1024, BF16)
            memT3 = memT.v("p (k m) -> p k m", m=256)
            m0 = AR.mark()
            stg = [AR.alloc(1024, F32), AR.alloc(1024, F32)]
            si = 0
            for (wm, o_ap, is_k) in ((w_mem_k[l], o_memk[l], True), (w_mem_v[l], o_memv[l], False)):
                for nb in range(2):
                    wb, wbufs = wget(wv(wm, 0, 8, nb * 512, 512), 8, 512)
                    if is_k:
                        for oc in range(4):
                            bk = PS()
                            mm(bk.f[:, 0:256], [(wb[:, k, oc * 128:(oc + 1) * 128], memT3[:, k, :]) for k in range(8)], wbufs + memT.b(), bk)
                            dst = kT.v("p (c m) -> p c m", m=256)[:, nb * 4 + oc, :]
                            S.op(ACT, lambda e, dst=dst, bk=bk: e.copy(dst, bk.f[:, 0:256]), reads=bk.buf, writes=kT.b())
                    ckpt('m1')
                    for a in range(2):
                        bk = PS()
                        mm(bk.f, [(memT3[:, k, a * 128:(a + 1) * 128], wb[:, k, :]) for k in range(8)], wbufs + memT.b(), bk)
                        ckpt('m2')
                        sg = stg[si % 2]; si += 1
                        S.op(ACT, lambda e, sg=sg, bk=bk: e.copy(sg.ap[:, 0:512], bk.f), reads=bk.buf, writes=sg.b())
                        if not is_k:
                            dst = vtk.ap[:, a * 1024 + nb * 512: a * 1024 + (nb + 1) * 512]
                            S.op(ACT, lambda e, dst=dst, bk=bk: e.copy(dst, bk.f), reads=bk.buf, writes=vtk.b())
                        S.op(ACT, lambda e, sg=sg, o_ap=o_ap, a=a, nb=nb: e.dma_start(out=o_ap[a * 128:(a + 1) * 128, nb * 512:(nb + 1) * 512], in_=sg.ap[:, 0:512]),
                             reads=sg.b(), dma_key=f"st{(si - 1) % 2}")
                        ckpt('m3')
                    ckpt('m4')
                ckpt('m5')
            AR.release(m0)

            hT = AR.alloc(2048, F32)
            hTb = AR.alloc(2048, BF16)
            hal = AR.alloc(24 * 3, F32)
            memset(hal, 0.0)

            ckpt(f'params{l}')
            gm = AR.mark()
            groups = [list(range(g * 4, g * 4 + 4)) for g in range(4)] + [[16]]
            for gi, tiles in enumerate(groups):
                ckpt(f'pre_g{l}_{gi}')
                AR.release(gm)
                mixer_group(l, W, x_src, x_mid, tiles, gi == 4, gi == 0, gi == 3,
                            dict(vg=vg, vb=vb, l1g=l1g, l1b=l1b, dtb=dtb, aneg=aneg, dsk=dsk, wdt=wdt, cw=cw, gss=gss,
                                 WmT=WmT, WsT=WsT, bsh=bsh, bsl=bsl, bshs=bshs, bsls=bsls, kT=kT, vtk=vtk, hT=hT, hTb=hTb, hal=hal))

        def gelu2_from_psum(bk, n, t1, t2, dst_ap, dst_bufs):
            p = bk.f[:, 0:n]
            a1 = t1.ap[:, 0:n]
            a2 = t2.ap[:, 0:n]
            S.op(ACT, lambda e: e.activation(a1, p, AF.Square), reads=bk.buf, writes=t1.b())
            S.op(DVE, lambda e: e.scalar_tensor_tensor(a1, a1, 0.044715, p, ALU.mult, ALU.mult), reads=bk.buf + t1.b(), writes=t1.b())
            S.op(DVE, lambda e: e.tensor_tensor(a1, a1, p, ALU.add), reads=bk.buf + t1.b(), writes=t1.b())
            S.op(ACT, lambda e: e.activation(a2, a1, AF.Tanh, scale=GELU_C), reads=t1.b(), writes=t2.b())
            S.op(DVE, lambda e: e.scalar_tensor_tensor(dst_ap, a2, 1.0, p, ALU.add, ALU.mult), reads=bk.buf + t2.b(), writes=dst_bufs)

        def silu2_from_psum(bk, n, t1, dst_ap, dst_bufs):
            p = bk.f[:, 0:n]
            a1 = t1.ap[:, 0:n]
            S.op(ACT, lambda e: e.activation(a1, p, AF.Tanh, scale=0.5), reads=bk.buf, writes=t1.b())
            S.op(DVE, lambda e: e.scalar_tensor_tensor(dst_ap, a1, 1.0, p, ALU.add, ALU.mult), reads=bk.buf + t1.b(), writes=dst_bufs)

        def gate_from_psum(bk, n, t1, fac):
            p = bk.f[:, 0:n]
            a1 = t1.ap[:, 0:n]
            S.op(ACT, lambda e: e.activation(a1, p, AF.Tanh, scale=0.5), reads=bk.buf, writes=t1.b())
            S.op(DVE, lambda e: e.tensor_scalar(a1, a1, 1.0, fac, ALU.add, ALU.mult), reads=t1.b(), writes=t1.b())

        def mixer_group(l, W, x_src, x_mid, tiles, is_s, first, last_prompt, P):
            ntl = len(tiles)
            T = 128 * ntl
            row0 = tiles[0] * 128
            xT = AR.alloc(8 * T, BF16)
            xT3 = xT.v("p (k t) -> p k t", t=T)
            mg = AR.alloc(8 * T, F32)
            mg3 = mg.v("p (k t) -> p k t", t=T)
            t1 = AR.alloc(512, F32); t2 = AR.alloc(512, F32); t3 = AR.alloc(512, F32)
            sm = AR.alloc(64, F32)
            m0 = AR.mark()
            xs2 = [AR.alloc(1024, F32), AR.alloc(1024, F32)]
            for ti, t in enumerate(tiles):
                xs_ = xs2[ti % 2]
                load(SP, xs_.b(), xs_.ap, x_src[t * 128:(t + 1) * 128, :], f"ldx{ti % 2}")
                for k4 in range(2):
                    bk = PS()
                    for kk in range(4):
                        k = k4 * 4 + kk
                        S.op(PE, lambda e, k=k, kk=kk, bk=bk, xs_=xs_: e.transpose(bk.f[:, kk * 128:(kk + 1) * 128], xs_.ap[:, k * 128:(k + 1) * 128], ident.ap),
                             reads=xs_.b() + ident.b(), writes=bk.buf)
                    dst = xT3[:, k4 * 4:(k4 + 1) * 4, ti * 128:(ti + 1) * 128]
                    S.op(ACT, lambda e, dst=dst, bk=bk: e.copy(dst, bk.f.rearrange("p (k t) -> p k t", t=128)), reads=bk.buf, writes=xT.b())
            AR.release(m0)

            gst = AR.alloc(4 * T, F32)
            gst3 = gst.v("p (c t) -> p c t", t=T)

            def project_merge(gcol, fac, y3, nk, pw, first_branch):
                for c4 in range(2):
                    gw, gwb = wget(wv(W, 0, 8, gcol + c4 * 512, 512), 8, 512)
                    for cc in range(4):
                        bk = PS()
                        mm(bk.f[:, 0:T], [(gw[:, k, cc * 128:(cc + 1) * 128], xT3[:, k, :]) for k in range(8)], gwb + xT.b(), bk)
                        S.op(ACT, lambda e, cc=cc, bk=bk: e.activation(gst3[:, cc, :], bk.f[:, 0:T], AF.Tanh, scale=0.5), reads=bk.buf, writes=gst.b(cc * T, (cc + 1) * T))
                        S.op(DVE, lambda e, cc=cc: e.tensor_scalar(gst3[:, cc, :], gst3[:, cc, :], 1.0, fac, ALU.add, ALU.mult), reads=gst.b(cc * T, (cc + 1) * T), writes=gst.b(cc * T, (cc + 1) * T))
                    for cc in range(4):
                        c = c4 * 4 + cc
                        if nk == 8:
                            if cc == 0:
                                pwa, pwb = wget(wv(pw, 0, 8, c * 128, 512), 8, 512)
                            co = cc * 128
                        else:
                            if cc % 2 == 0:
                                pwa, pwb = wget(wv(pw, 0, 16, c * 128, 256), 16, 256)
                            co = (cc % 2) * 128
                        bk2 = PS()
                        mm(bk2.f[:, 0:T], [(pwa[:, k, co:co + 128], y3[0][:, k, :]) for k in range(nk)], pwb + y3[1], bk2)
                        mgb = mg.b(c * T, (c + 1) * T)
                        if first_branch:
                            S.op(DVE, lambda e, c=c, cc=cc, bk2=bk2: e.tensor_tensor(mg3[:, c, :], gst3[:, cc, :], bk2.f[:, 0:T], ALU.mult), reads=bk2.buf + gst.b(cc * T, (cc + 1) * T), writes=mgb)
                        else:
                            S.op(DVE, lambda e, cc=cc, bk2=bk2: e.tensor_tensor(gst3[:, cc, :], gst3[:, cc, :], bk2.f[:, 0:T], ALU.mult), reads=bk2.buf + gst.b(cc * T, (cc + 1) * T), writes=gst.b(cc * T, (cc + 1) * T))
                            S.op(POOL, lambda e, c=c, cc=cc: e.tensor_tensor(mg3[:, c, :], mg3[:, c, :], gst3[:, cc, :], ALU.add), reads=gst.b(cc * T, (cc + 1) * T) + mgb, writes=mgb)

            bm = AR.mark()
            load(SP, P["vg"].b(), P["vg"].ap, v_ln_g[l:l + 1, :].partition_broadcast(128), "ldp")
            load(SP, P["vb"].b(), P["vb"].ap, v_ln_b[l:l + 1, :].partition_broadcast(128), "ldp")
            uT = AR.alloc(8 * T, BF16)
            uT3 = uT.v("p (k t) -> p k t", t=T)
            vtok = AR.alloc(ntl * 1024, BF16)
            vtmp = AR.alloc(1024, F32)
            vout = AR.alloc(1024, F32)
            for c4 in range(2):
                wb, wbufs = wget(wv(W, 0, 8, C_U + c4 * 512, 512), 8, 512)
                for cc in range(4):
                    c = c4 * 4 + cc
                    bk = PS()
                    mm(bk.f[:, 0:T], [(wb[:, k, cc * 128:(cc + 1) * 128], xT3[:, k, :]) for k in range(8)], wbufs + xT.b(), bk)
                    gelu2_from_psum(bk, T, t1, t2, uT3[:, c, :], uT.b(c * T, (c + 1) * T))
            vws = [wget(wv(W, 0, 8, C_V + nb * 512, 512), 8, 512) for nb in range(2)]
            for ti in range(ntl):
                for nb in range(2):
                    wb, wbufs = vws[nb]
                    bk = PS()
                    mm(bk.f, [(xT3[:, k, ti * 128:(ti + 1) * 128], wb[:, k, :]) for k in range(8)], wbufs + xT.b(), bk)
                    gelu2_from_psum(bk, 512, t1, t2, vtmp.ap[:, nb * 512:(nb + 1) * 512], vtmp.b(nb * 512, (nb + 1) * 512))
                dst = vtok.ap[:, ti * 1024:(ti + 1) * 1024]
                layer_norm(vtmp.b(), vtmp.ap, P["vg"], P["vb"], sm, 4 * EPS, out_ap=dst, out_tv=vtok.b(ti * 1024, (ti + 1) * 1024),
                           out2=(vout.b(), vout.ap) if is_s else None)
                if is_s:
                    S.op(ACT, lambda e: e.dma_start(out=o_gv[l], in_=vout.ap), reads=vout.b(), dma_key="st2")
                Wsp = P["WsT"] if is_s else P["WmT"]
                bh, bl = (P["bshs"], P["bsls"]) if is_s else (P["bsh"], P["bsl"])
                for g4 in range(2):
                    bk = PS()
                    for gg in range(4):
                        g = g4 * 4 + gg
                        mm(bk.f[:, gg * 128:(gg + 1) * 128],
                           [(vtok.ap[:, ti * 1024 + g * 128: ti * 1024 + (g + 1) * 128], Wsp.ap[:, g * 128:(g + 1) * 128]),
                            (ones1b.ap[0:1, :], bh.ap[0:1, g * 128:(g + 1) * 128]),
                            (ones1b.ap[0:1, :], bl.ap[0:1, g * 128:(g + 1) * 128])],
                           vtok.b(ti * 1024, (ti + 1) * 1024) + Wsp.b() + ones1b.b() + bh.b() + bl.b(), bk)
                    dst = uT3[:, g4 * 4:(g4 + 1) * 4, ti * 128:(ti + 1) * 128]
                    S.op(DVE, lambda e, dst=dst, bk=bk: e.tensor_tensor(dst, dst, bk.f.rearrange("p (g i) -> p g i", i=128), ALU.mult), reads=bk.buf + uT.b(), writes=uT.b())
            project_merge(C_G, 0.25, (uT3, uT.b()), 8, p_gm[l], True)
            AR.release(bm)
            ckpt(f'A{l}_{tiles[0]}')

            qT = AR.alloc(8 * T, BF16)
            qT3 = qT.v("p (k t) -> p k t", t=T)
            yx = AR.alloc(8 * T, BF16)
            yx3 = yx.v("p (k t) -> p k t", t=T)
            for c4 in range(2):
                wb, wbufs = wget(wv(W, 0, 8, C_Q + c4 * 512, 512), 8, 512)
                for cc in range(4):
                    c = c4 * 4 + cc
                    bk = PS()
                    mm(bk.f[:, 0:T], [(wb[:, k, cc * 128:(cc + 1) * 128], xT3[:, k, :]) for k in range(8)], wbufs + xT.b(), bk)
                    S.op(ACT, lambda e, c=c, bk=bk: e.activation(qT3[:, c, :], bk.f[:, 0:T], AF.Copy, scale=0.0625), reads=bk.buf, writes=qT.b(c * T, (c + 1) * T))
            if not is_s:
                es = AR.alloc(8 * T, BF16)
                es4 = es.v("p (a h t) -> p a h t", a=2, h=4)
                rden = AR.alloc(T, F32)
                kT3 = P["kT"].v("p (c m) -> p c m", m=256)
                for h in range(4):
                    for a in range(2):
                        bk = PS()
                        mm(bk.f[:, 0:T], [(kT3[:, 2 * h + hf, a * 128:(a + 1) * 128], qT3[:, 2 * h + hf, :]) for hf in range(2)], P["kT"].b() + qT.b(), bk)
                        S.op(ACT, lambda e, h=h, a=a, bk=bk: e.activation(es4[:, a, h, :], bk.f[:, 0:T], AF.Exp), reads=bk.buf, writes=es.b((a * 4 + h) * T, (a * 4 + h + 1) * T))
                    bkd = PS()
                    mm(bkd.f[:, 0:T], [(onesb.ap, es4[:, a, h, :]) for a in range(2)], onesb.b() + es.b(), bkd)
                    S.op(DVE, lambda e, bkd=bkd: e.reciprocal(rden.ap, bkd.f[:, 0:T]), reads=bkd.buf, writes=rden.b())
                    for dc in range(2):
                        bk = PS()
                        mm(bk.f[:, 0:T], [(P["vtk"].ap[:, a * 1024 + h * 256 + dc * 128: a * 1024 + h * 256 + (dc + 1) * 128], es4[:, a, h, :]) for a in range(2)], P["vtk"].b() + es.b(), bk)
                        S.op(DVE, lambda e, h=h, dc=dc, bk=bk: e.tensor_tensor(yx3[:, 2 * h + dc, :], bk.f[:, 0:T], rden.ap, ALU.mult), reads=bk.buf + rden.b(), writes=yx.b((2 * h + dc) * T, (2 * h + dc + 1) * T))
            else:
                kb2 = [AR.alloc(2048, BF16), AR.alloc(2048, BF16)]
                vb2 = [AR.alloc(2048, BF16), AR.alloc(2048, BF16)]
                kTb = AR.alloc(8 * 256, BF16)
                kTb3 = kTb.v("p (c m) -> p c m", m=256)
                esb = AR.alloc(64, BF16)
                esb4 = esb.v("p (a h i) -> p a h i", a=2, h=4)
                rdb = AR.alloc(32, F32)
                for b in range(16):
                    kb = kb2[b % 2]; vbb = vb2[b % 2]
                    for a in range(2):
                        stage_cast(kb.ap[:, a * 1024:(a + 1) * 1024], kb.b(a * 1024, (a + 1) * 1024), ck[l, b, a * 128:(a + 1) * 128, :], [128, 1, 1024])
                        stage_cast(vbb.ap[:, a * 1024:(a + 1) * 1024], vbb.b(a * 1024, (a + 1) * 1024), cv[l, b, a * 128:(a + 1) * 128, :], [128, 1, 1024])
                    for a in range(2):
                        bk = PS()
                        for c in range(8):
                            S.op(PE, lambda e, a=a, c=c, bk=bk, kb=kb: e.transpose(bk.h[:, c * 128:(c + 1) * 128], kb.ap[:, a * 1024 + c * 128: a * 1024 + (c + 1) * 128], identb.ap),
                                 reads=kb.b() + identb.b(), writes=bk.buf)
                        S.op(ACT, lambda e, a=a, bk=bk: e.copy(kTb3[:, :, a * 128:(a + 1) * 128], bk.h.rearrange("p (c m) -> p c m", m=128)), reads=bk.buf, writes=kTb.b())
                    bk = PS()
                    for a in range(2):
                        for h in range(4):
                            mm(bk.f[:, (a * 4 + h) * 8:(a * 4 + h + 1) * 8], [(kTb3[:, 2 * h + hf, a * 128:(a + 1) * 128], qT3[:, 2 * h + hf, b * 8:(b + 1) * 8]) for hf in range(2)], kTb.b() + qT.b(), bk)
                    S.op(ACT, lambda e, bk=bk: e.activation(esb.ap, bk.f[:, 0:64], AF.Exp), reads=bk.buf, writes=esb.b())
                    bkd = PS()
                    mm(bkd.f[:, 0:32], [(onesb.ap, esb.ap[:, a * 32:(a + 1) * 32]) for a in range(2)], onesb.b() + esb.b(), bkd)
                    S.op(DVE, lambda e, bkd=bkd: e.reciprocal(rdb.ap, bkd.f[:, 0:32]), reads=bkd.buf, writes=rdb.b())
                    bk = PS()
                    for h in range(4):
                        for dc in range(2):
                            mm(bk.f[:, (2 * h + dc) * 8:(2 * h + dc + 1) * 8], [(vbb.ap[:, a * 1024 + h * 256 + dc * 128: a * 1024 + h * 256 + (dc + 1) * 128], esb4[:, a, h, :]) for a in range(2)], vbb.b() + esb.b(), bk)
                    src = bk.f[:, 0:64].rearrange("p (h d i) -> p h d i", h=4, d=2)
                    rd4 = rdb.v("p (h i) -> p h i", i=8).unsqueeze(2).to_broadcast([128, 4, 2, 8])
                    dst = yx3[:, :, b * 8:(b + 1) * 8].rearrange("p (h d) i -> p h d i", d=2)
                    S.op(DVE, lambda e, dst=dst, src=src, rd4=rd4: e.tensor_tensor(dst, src, rd4, ALU.mult), reads=bk.buf + rdb.b(), writes=yx.b())
            project_merge(C_G + 2048, 0.5, (yx3, yx.b()), 8, p_xa[l], False)
            AR.release(bm)
            ckpt(f'C{l}_{tiles[0]}')

            ssd_branch(l, W, tiles, is_s, first, last_prompt, P, xT, xT3, T, t1, t2, t3, sm, project_merge)
            AR.release(bm)
            ckpt(f'B{l}_{tiles[0]}')

            load(SP, P["l1g"].b(), P["l1g"].ap, ln1_g[l:l + 1, :].partition_broadcast(128), "ldp")
            load(SP, P["l1b"].b(), P["l1b"].ap, ln1_b[l:l + 1, :].partition_broadcast(128), "ldp")
            mb = AR.alloc(8 * T, BF16)
            mb3 = mb.v("p (k t) -> p k t", t=T)
            for c in range(8):
                S.op(ACT, lambda e, c=c: e.copy(mb3[:, c, :], mg3[:, c, :]), reads=mg.b(c * T, (c + 1) * T), writes=mb.b(c * T, (c + 1) * T))
            xr = [AR.alloc(1024, F32) for _ in range(ntl)]
            for ti, t in enumerate(tiles):
                load(SP, xr[ti].b(), xr[ti].ap, x_src[t * 128:(t + 1) * 128, :], f"ldr{ti}")
            for nb in range(2):
                wb, wbufs = wget(wv(w_out[l], 0, 8, nb * 512, 512), 8, 512)
                for ti in range(ntl):
                    bk = PS()
                    mm(bk.f, [(mb3[:, k, ti * 128:(ti + 1) * 128], wb[:, k, :]) for k in range(8)], wbufs + mb.b(), bk)
                    xa = xr[ti].ap[:, nb * 512:(nb + 1) * 512]
                    S.op(DVE, lambda e, xa=xa, bk=bk: e.scalar_tensor_tensor(xa, xa, ALPHA, bk.f, ALU.mult, ALU.add), reads=bk.buf + xr[ti].b(nb * 512, (nb + 1) * 512), writes=xr[ti].b(nb * 512, (nb + 1) * 512))
            for ti, t in enumerate(tiles):
                layer_norm(xr[ti].b(), xr[ti].ap, P["l1g"], P["l1b"], sm, EPS)
                S.op(ACT, lambda e, ti=ti, t=t: e.dma_start(out=x_mid[t * 128:(t + 1) * 128, :], in_=xr[ti].ap), reads=xr[ti].b(), dma_key=f"stx{ti}")

        def ssd_branch(l, W, tiles, is_s, first, last_prompt, P, xT, xT3, T, t1, t2, t3, sm, project_merge):
            ntl = len(tiles)
            sz = AR.alloc(ntl * 2048, BF16)
            xc = AR.alloc(24 * T, BF16)
            xc3 = xc.v("p (c t) -> p c t", t=T)
            dts = AR.alloc(ntl * 32, F32)
            adt = AR.alloc(ntl * 32, F32)
            cw3 = P["cw"].v("p (c r) -> p c r", r=5)
            for nb in range(4):
                wb, wbufs = wget(wv(W, 0, 8, C_Z + nb * 512, 512), 8, 512)
                for ti in range(ntl):
                    bk = PS()
                    mm(bk.f, [(xT3[:, k, ti * 128:(ti + 1) * 128], wb[:, k, :]) for k in range(8)], wbufs + xT.b(), bk)
                    silu2_from_psum(bk, 512, t1, sz.ap[:, ti * 2048 + nb * 512: ti * 2048 + (nb + 1) * 512], sz.b(ti * 2048 + nb * 512, ti * 2048 + (nb + 1) * 512))
            ckpt('Bz')
            bk = PS()
            wdt3 = P["wdt"].v("p (k n) -> p k n", n=32)
            for ti in range(ntl):
                mm(bk.f[:, ti * 32:(ti + 1) * 32], [(xT3[:, k, ti * 128:(ti + 1) * 128], wdt3[:, k, :]) for k in range(8)], P["wdt"].b() + xT.b(), bk)
            n32 = ntl * 32
            xa = t1.ap[:, 0:n32]; na = t2.ap[:, 0:n32]
            S.op(DVE, lambda e: e.tensor_tensor(xa.rearrange("p (t r) -> p t r", r=32), bk.f[:, 0:n32].rearrange("p (t r) -> p t r", r=32),
                                                P["dtb"].ap.unsqueeze(1).to_broadcast([128, ntl, 32]), ALU.add), reads=bk.buf + P["dtb"].b(), writes=t1.b())
            S.op(DVE, lambda e: e.scalar_tensor_tensor(na, xa, -1.0, xa, ALU.mult, ALU.min), reads=t1.b(), writes=t2.b())
            S.op(ACT, lambda e: e.activation(na, na, AF.Exp), reads=t2.b(), writes=t2.b())
            S.op(ACT, lambda e: e.activation(na, na, AF.Ln, bias=1.0), reads=t2.b(), writes=t2.b())
            S.op(DVE, lambda e: e.scalar_tensor_tensor(dts.ap, xa, 0.0, na, ALU.max, ALU.add), reads=t1.b() + t2.b(), writes=dts.b())
            S.op(DVE, lambda e: e.tensor_tensor(adt.v("p (t r) -> p t r", r=32), dts.v("p (t r) -> p t r", r=32),
                                                P["aneg"].ap.unsqueeze(1).to_broadcast([128, ntl, 32]), ALU.mult), reads=dts.b() + P["aneg"].b(), writes=adt.b())
            ckpt('Bdt')
            m0 = AR.mark()
            hal3 = P["hal"].v("p (c r) -> p c r", r=3)
            if is_s:
                srows = AR.alloc(3072, F32)
                load(SP, srows.b(), srows.ap[0:48, :], sconv[l], "ldp")
                halS = AR.alloc(24 * 48, F32)
                rows_to_cols(srows.b(), srows.ap, 48, 24, halS.b(), halS.v("p (c r) -> p c r", r=48))
                newS = AR.alloc(24 * 48, F32)
                W_ = 16 * 11
            else:
                W_ = T + 3
            cb2 = [AR.alloc(W_, F32), AR.alloc(W_, F32)]
            acc2 = [AR.alloc(T, F32), AR.alloc(T, F32)]
            for c4 in range(6):
                wb, wbufs = wget(wv(W, 0, 8, C_XBC + c4 * 512, 512), 8, 512)
                for cc in range(4):
                    c = c4 * 4 + cc
                    bk = PS()
                    mm(bk.f[:, 0:T], [(wb[:, k, cc * 128:(cc + 1) * 128], xT3[:, k, :]) for k in range(8)], wbufs + xT.b(), bk)
                    cbf = cb2[c % 2]; acc = acc2[c % 2]
                    if is_s:
                        c3 = cbf.v("p (b j) -> p b j", j=11)
                        S.op(ACT, lambda e, c3=c3, bk=bk: e.copy(c3[:, :, 3:11], bk.f[:, 0:128].rearrange("p (b i) -> p b i", i=8)), reads=bk.buf, writes=cbf.b())
                        hs = halS.v("p (c b r) -> p c b r", b=16, r=3)[:, c, :, :]
                        S.op(POOL, lambda e, c3=c3, hs=hs: e.tensor_copy(c3[:, :, 0:3], hs), reads=halS.b(), writes=cbf.b())
                        ns = newS.v("p (c b r) -> p c b r", b=16, r=3)[:, c, :, :]
                        S.op(POOL, lambda e, c3=c3, ns=ns: e.tensor_copy(ns, c3[:, :, 8:11]), reads=cbf.b(), writes=newS.b())
                        a3 = acc.v("p (b i) -> p b i", i=8)
                        taps = [c3[:, :, k:k + 8] for k in range(4)]
                        accv = a3
                    else:
                        S.op(ACT, lambda e, cbf=cbf, bk=bk: e.copy(cbf.ap[:, 3:3 + T], bk.f[:, 0:T]), reads=bk.buf, writes=cbf.b())
                        S.op(POOL, lambda e, cbf=cbf, c=c: e.tensor_copy(cbf.ap[:, 0:3], hal3[:, c, :]), reads=P["hal"].b(), writes=cbf.b())
                        S.op(POOL, lambda e, cbf=cbf, c=c: e.tensor_copy(hal3[:, c, :], cbf.ap[:, T:T + 3]), reads=cbf.b(), writes=P["hal"].b())
                        taps = [cbf.ap[:, k:k + T] for k in range(4)]
                        accv = acc.ap
                    S.op(DVE, lambda e, accv=accv, taps=taps, c=c: e.tensor_scalar(accv, taps[0], cw3[:, c, 0:1], cw3[:, c, 4:5], ALU.mult, ALU.add), reads=cbf.b() + P["cw"].b(), writes=acc.b())
                    for k in range(1, 4):
                        S.op(DVE, lambda e, accv=accv, taps=taps, c=c, k=k: e.scalar_tensor_tensor(accv, taps[k], cw3[:, c, k:k + 1], accv, ALU.mult, ALU.add), reads=cbf.b() + P["cw"].b() + acc.b(), writes=acc.b())
                    S.op(ACT, lambda e, acc=acc: e.activation(t3.ap[:, 0:T], acc.ap, AF.Tanh), reads=acc.b(), writes=t3.b())
                    S.op(DVE, lambda e, acc=acc, c=c: e.scalar_tensor_tensor(xc3[:, c, :], t3.ap[:, 0:T], 1.0, acc.ap, ALU.add, ALU.mult), reads=acc.b() + t3.b(), writes=xc.b(c * T, (c + 1) * T))
            if is_s:
                cols_to_rows(newS.b(), newS.v("p (c r) -> p c r", r=48), 48, 24, srows.b(), srows.ap)
                S.op(ACT, lambda e: e.dma_start(out=o_convs[l], in_=srows.ap[0:48, :]), reads=srows.b(), dma_key="st3")
            elif last_prompt:
                prow = AR.alloc(3072, F32)
                cols_to_rows(P["hal"].b(), hal3, 3, 24, prow.b(), prow.ap)
                S.op(ACT, lambda e: e.dma_start(out=o_convp[l], in_=prow.ap[0:3, :]), reads=prow.b(), dma_key="st3")
            AR.release(m0)

            ckpt('Bconv')
            tm = AR.mark()
            xst = AR.alloc(2048, BF16)
            xdt = AR.alloc(2048, BF16)
            xde = AR.alloc(512, BF16)
            btk = AR.alloc(512, BF16)
            ex = AR.alloc(96, F32)
            rr = AR.alloc(8 * 128, F32)
            dec = AR.alloc(8 * 128, BF16)
            cbm = AR.alloc(128, BF16)
            MT = AR.alloc(8 * 128, BF16)
            yt = AR.alloc(512, F32)
            yb = AR.alloc(512, BF16)
            hT, hTb = P["hT"], P["hTb"]
            mU = UBD if is_s else maskU
            mL = LBD if is_s else Lst
            mO = BD if is_s else ones
            ftmp = AR.alloc(512, F32)
            if is_s:
                ydg = [AR.alloc(512, F32) for _ in range(4)]
                dcol = AR.alloc(256, F32)
                totb = AR.alloc(512, F32)
            for ti in range(ntl):
                tc0 = ti * 128
                chunk0 = first and ti == 0
                dt_t = dts.ap[:, ti * 32:(ti + 1) * 32]
                adt_t = adt.ap[:, ti * 32:(ti + 1) * 32]
                for h2 in range(2):
                    bk = PS()
                    for c in range(8):
                        S.op(PE, lambda e, c=c, h2=h2, bk=bk: e.transpose(bk.h[:, c * 128:(c + 1) * 128], xc3[:, h2 * 8 + c, tc0:tc0 + 128], identb.ap),
                             reads=xc.b() + identb.b(), writes=bk.buf)
                    S.op(ACT, lambda e, h2=h2, bk=bk: e.copy(xst.ap[:, h2 * 1024:(h2 + 1) * 1024], bk.h), reads=bk.buf, writes=xst.b(h2 * 1024, (h2 + 1) * 1024))
                    ckpt('Ba1')
                    S.op(DVE, lambda e, h2=h2: e.tensor_tensor(xdt.ap[:, h2 * 1024:(h2 + 1) * 1024].rearrange("p (r d) -> p r d", d=64), xst.ap[:, h2 * 1024:(h2 + 1) * 1024].rearrange("p (r d) -> p r d", d=64),
                                                              dt_t[:, h2 * 16:(h2 + 1) * 16].unsqueeze(2).to_broadcast([128, 16, 64]), ALU.mult),
                         reads=xst.b(h2 * 1024, (h2 + 1) * 1024) + dts.b(), writes=xdt.b(h2 * 1024, (h2 + 1) * 1024))
                    ckpt('Ba2')
                bk = PS()
                for g in range(4):
                    S.op(PE, lambda e, g=g, bk=bk: e.transpose(bk.h[:, g * 128:(g + 1) * 128], xc3[:, 16 + g, tc0:tc0 + 128], identb.ap), reads=xc.b() + identb.b(), writes=bk.buf)
                S.op(ACT, lambda e, bk=bk: e.copy(btk.ap, bk.h[:, 0:512]), reads=bk.buf, writes=btk.b())
                ckpt('Ba')
                bk = PS()
                mm(bk.f[:, 0:32], [(mU.ap, adt_t)], mU.b() + adt.b(), bk)
                mm(bk.f[:, 32:64], [(mL.ap, adt_t)], mL.b() + adt.b(), bk)
                mm(bk.f[:, 64:96], [(mO.ap, adt_t)], mO.b() + adt.b(), bk)
                S.op(ACT, lambda e, bk=bk: e.activation(ex.ap, bk.f[:, 0:96], AF.Exp), reads=bk.buf, writes=ex.b())
                if is_s:
                    S.op(DVE, lambda e, bk=bk: e.tensor_tensor(totb.v("p (b r) -> p b r", r=32), bk.f[:, 64:96].unsqueeze(1).to_broadcast([128, 16, 32]),
                                                              onehot.ap.unsqueeze(2).to_broadcast([128, 16, 32]), ALU.mult), reads=bk.buf + onehot.b(), writes=totb.b())
                    bk2 = PS()
                    mm(bk2.f, [(eighth.ap, totb.ap)], eighth.b() + totb.b(), bk2)
                    s4 = bk2.f.rearrange("p (b q w) -> p b q w", q=16, w=2)
                    d3 = dcol.v("p (b q) -> p b q", q=16)
                    S.op(ACT, lambda e, s4=s4, d3=d3: e.activation(d3[0:64], s4[0:64, :, :, 0], AF.Exp), reads=bk2.buf, writes=dcol.b())
                    S.op(ACT, lambda e, s4=s4, d3=d3: e.activation(d3[64:128], s4[64:128, :, :, 1], AF.Exp), reads=bk2.buf, writes=dcol.b())
                ckpt('Bb')
                for g in range(4):
                    BTg = xc3[:, 16 + g, tc0:tc0 + 128]
                    CTg = xc3[:, 20 + g, tc0:tc0 + 128]
                    S.op(POOL, lambda e, g=g: e.tensor_tensor(rr.v("p (r i) -> p r i", i=128), mU.ap.unsqueeze(1).to_broadcast([128, 8, 128]),
                                                              adt_t[:, g * 8:(g + 1) * 8].unsqueeze(2).to_broadcast([128, 8, 128]), ALU.mult), reads=mU.b() + adt.b(), writes=rr.b())
                    for h2 in range(2):
                        bk = PS()
                        mm(bk.f, [(mL.ap, rr.ap[:, h2 * 512:(h2 + 1) * 512])], mL.b() + rr.b(), bk)
                        S.op(ACT, lambda e, h2=h2, bk=bk: e.activation(dec.ap[:, h2 * 512:(h2 + 1) * 512], bk.f, AF.Exp), reads=bk.buf, writes=dec.b(h2 * 512, (h2 + 1) * 512))
                    bk = PS()
                    mm(bk.f[:, 0:128], [(BTg, CTg)], xc.b(), bk)
                    S.op(DVE, lambda e, bk=bk: e.tensor_tensor(cbm.ap, bk.f[:, 0:128], mU.ap, ALU.mult), reads=bk.buf + mU.b(), writes=cbm.b())
                    S.op(DVE, lambda e: e.tensor_tensor(MT.v("p (r i) -> p r i", i=128), dec.v("p (r i) -> p r i", i=128), cbm.ap.unsqueeze(1).to_broadcast([128, 8, 128]), ALU.mult),
                         reads=dec.b() + cbm.b(), writes=MT.b())
                    ckpt('Bc')
                    bky = PS()
                    for r in range(8):
                        mm(bky.f[:, r * 64:(r + 1) * 64], [(MT.ap[:, r * 128:(r + 1) * 128], xdt.ap[:, (g * 8 + r) * 64:(g * 8 + r + 1) * 64])], MT.b() + xdt.b(), bky)
                    if is_s:
                        S.op(ACT, lambda e, g=g, bky=bky: e.copy(ydg[g].ap, bky.f), reads=bky.buf, writes=ydg[g].b())
                        continue
                    have_off = not chunk0
                    if have_off:
                        bko = PS()
                        mm(bko.f, [(CTg, hTb.ap[:, g * 512:(g + 1) * 512])], xc.b() + hTb.b(), bko)
                    eac = ex.ap[:, g * 8:(g + 1) * 8].unsqueeze(2).to_broadcast([128, 8, 64])
                    y3v = yt.v("p (r d) -> p r d", d=64)
                    if have_off:
                        S.op(DVE, lambda e, bko=bko, eac=eac, y3v=y3v: e.tensor_tensor(y3v, bko.f.rearrange("p (r d) -> p r d", d=64), eac, ALU.mult), reads=bko.buf + ex.b(), writes=yt.b())
                        S.op(DVE, lambda e, bky=bky: e.tensor_tensor(yt.ap, yt.ap, bky.f, ALU.add), reads=bky.buf + yt.b(), writes=yt.b())
                    else:
                        S.op(DVE, lambda e, bky=bky: e.tensor_copy(yt.ap, bky.f), reads=bky.buf, writes=yt.b())
                    ckpt('Bd')
                    tee = ex.ap[:, 32 + g * 8: 32 + (g + 1) * 8].unsqueeze(2).to_broadcast([128, 8, 64])
                    S.op(POOL, lambda e, g=g, tee=tee: e.tensor_tensor(xde.v("p (r d) -> p r d", d=64), xdt.ap[:, g * 512:(g + 1) * 512].rearrange("p (r d) -> p r d", d=64), tee, ALU.mult),
                         reads=xdt.b(g * 512, (g + 1) * 512) + ex.b(), writes=xde.b())
                    bks = PS()
                    mm(bks.f, [(btk.ap[:, g * 128:(g + 1) * 128], xde.ap)], btk.b() + xde.b(), bks)
                    hg = hT.ap[:, g * 512:(g + 1) * 512]
                    hgb = hT.b(g * 512, (g + 1) * 512)
                    if chunk0:
                        S.op(DVE, lambda e, hg=hg, bks=bks: e.tensor_copy(hg, bks.f), reads=bks.buf, writes=hgb)
                    else:
                        dcy = ex.ap[:, 64 + g * 8: 64 + (g + 1) * 8].unsqueeze(2).to_broadcast([128, 8, 64])
                        S.op(DVE, lambda e, hg=hg, dcy=dcy: e.tensor_tensor(hg.rearrange("p (r d) -> p r d", d=64), hg.rearrange("p (r d) -> p r d", d=64), dcy, ALU.mult), reads=hgb + ex.b(), writes=hgb)
                        S.op(DVE, lambda e, hg=hg, bks=bks: e.tensor_tensor(hg, hg, bks.f, ALU.add), reads=bks.buf + hgb, writes=hgb)
                    S.op(ACT, lambda e, hg=hg, g=g: e.copy(hTb.ap[:, g * 512:(g + 1) * 512], hg), reads=hgb, writes=hTb.b(g * 512, (g + 1) * 512))
                    ckpt('Be')
                    ssd_finish_group(l, P, g, ti, T, tc0, xst, sz, yt, yb, xc, xc3, sm, ftmp)
                    ckpt('Bf')
                if is_s:
                    ssd_sample_tail(l, P, T, xst, sz, yt, yb, xc, xc3, sm, xdt, btk, ex, ydg, dcol, ftmp)
            if last_prompt:
                AR.release(tm)
                m1 = AR.mark()
                hst = AR.alloc(2048, F32)
                for q4 in range(4):
                    bk = PS()
                    for qq in range(4):
                        q = q4 * 4 + qq
                        S.op(PE, lambda e, q=q, qq=qq, bk=bk: e.transpose(bk.f[:, qq * 128:(qq + 1) * 128], hT.ap[:, q * 128:(q + 1) * 128], ident.ap), reads=hT.b() + ident.b(), writes=bk.buf)
                    S.op(ACT, lambda e, q4=q4, bk=bk: e.copy(hst.ap[:, q4 * 512:(q4 + 1) * 512], bk.f), reads=bk.buf, writes=hst.b(q4 * 512, (q4 + 1) * 512))
                S.op(ACT, lambda e: e.dma_start(out=o_ssmp[l].rearrange("(q w) p n -> (w p) q n", w=2), in_=hst.v("p (q n) -> p q n", n=128)), reads=hst.b(), dma_key="st4")
                AR.release(m1)
            project_merge(C_G + 1024, 0.5, (xc3, xc.b(0, 16 * T)), 16, p_ssd[l], False)

        def ssd_finish_group(l, P, g, ti, T, tc0, xst, sz, yt, yb, xc, xc3, sm, tmp):
            dsk = P["dsk"].ap[:, g * 8:(g + 1) * 8].unsqueeze(2).to_broadcast([128, 8, 64])
            xg = xst.ap[:, g * 512:(g + 1) * 512]
            S.op(POOL, lambda e: e.tensor_tensor(tmp.v("p (r d) -> p r d", d=64), xg.rearrange("p (r d) -> p r d", d=64), dsk, ALU.mult), reads=xst.b(g * 512, (g + 1) * 512) + P["dsk"].b(), writes=tmp.b())
            S.op(POOL, lambda e: e.tensor_tensor(yt.ap, yt.ap, tmp.ap, ALU.add), reads=tmp.b() + yt.b(), writes=yt.b())
            S.op(DVE, lambda e: e.tensor_tensor(yt.ap, yt.ap, sz.ap[:, ti * 2048 + g * 512: ti * 2048 + (g + 1) * 512], ALU.mult), reads=yt.b() + sz.b(ti * 2048 + g * 512, ti * 2048 + (g + 1) * 512), writes=yt.b())
            stats = sm.ap[:, 0:6]; mv = sm.ap[:, 6:8]; ms = sm.ap[:, 8:9]; rstd = sm.ap[:, 9:10]
            S.op(DVE, lambda e: e.bn_stats(stats, yt.ap), reads=yt.b(), writes=sm.b())
            S.op(DVE, lambda e: e.bn_aggr(mv, stats), reads=sm.b(), writes=sm.b())
            S.op(DVE, lambda e: e.scalar_tensor_tensor(ms, mv[:, 0:1], mv[:, 0:1], mv[:, 1:2], ALU.mult, ALU.add), reads=sm.b(), writes=sm.b())
            S.op(DVE, lambda e: e.tensor_scalar_add(ms, ms, 4 * EPS), reads=sm.b(), writes=sm.b())
            S.op(POOLX, lambda e: e.tensor_tensor(rstd, ms, mhalf.ap, ALU.pow), reads=sm.b() + mhalf.b(), writes=sm.b())
            S.op(DVE, lambda e: e.tensor_scalar_mul(yb.ap, yt.ap, rstd), reads=yt.b() + sm.b(), writes=yb.b())
            bk = PS()
            for c in range(4):
                S.op(PE, lambda e, c=c, bk=bk: e.transpose(bk.h[:, c * 128:(c + 1) * 128], yb.ap[:, c * 128:(c + 1) * 128], identb.ap), reads=yb.b() + identb.b(), writes=bk.buf)
            gs3 = P["gss"].ap
            for c in range(4):
                ch = g * 4 + c
                S.op(ACT, lambda e, c=c, ch=ch, bk=bk: e.activation(xc3[:, ch, tc0:tc0 + 128], bk.h[:, c * 128:(c + 1) * 128], AF.Copy, scale=gs3[:, ch:ch + 1]),
                     reads=bk.buf + P["gss"].b(), writes=xc.b())

        def ssd_sample_tail(l, P, T, xst, sz, yt, yb, xc, xc3, sm, xdt, btk, ex, ydg, dcol, ftmp):
            d3 = dcol.v("p (b q) -> p b q", q=16)
            tee = ex.ap[:, 32:64].unsqueeze(2).to_broadcast([128, 32, 64])
            S.op(POOL, lambda e: e.tensor_tensor(xdt.v("p (r d) -> p r d", d=64), xdt.v("p (r d) -> p r d", d=64), tee, ALU.mult), reads=xdt.b() + ex.b(), writes=xdt.b())
            h0f2 = [AR.alloc(2048, F32), AR.alloc(2048, F32)]
            h0b2 = [AR.alloc(2048, BF16), AR.alloc(2048, BF16)]
            h0T = AR.alloc(2048, BF16)
            hout = AR.alloc(2048, F32)
            bmk = AR.alloc(512, BF16)
            ctm = AR.alloc(512, BF16)
            yoff = [AR.alloc(512, F32) for _ in range(4)]
            for g in range(4):
                memset(yoff[g], 0.0)
            CT4 = xc3[:, 20:24, 0:128]
            cm3 = colmask.v("p (b i) -> p b i", i=128)
            for b in range(16):
                h0f = h0f2[b % 2]; h0b = h0b2[b % 2]
                src = sssm[l, b].rearrange("(q w) p n -> (w p) q n", w=2)
                load(SP, h0f.b(), h0f.v("p (q n) -> p q n", n=128), src, f"ldh{b % 2}")
                S.op(ACT, lambda e, h0f=h0f, h0b=h0b: e.copy(h0b.ap, h0f.ap), reads=h0f.b(), writes=h0b.b())
                for h2 in range(2):
                    bk = PS()
                    for q in range(8):
                        S.op(PE, lambda e, q=q, h2=h2, bk=bk, h0b=h0b: e.transpose(bk.h[:, q * 128:(q + 1) * 128], h0b.ap[:, (h2 * 8 + q) * 128:(h2 * 8 + q + 1) * 128], identb.ap),
                             reads=h0b.b() + identb.b(), writes=bk.buf)
                    S.op(ACT, lambda e, h2=h2, bk=bk: e.copy(h0T.ap[:, h2 * 1024:(h2 + 1) * 1024], bk.h), reads=bk.buf, writes=h0T.b(h2 * 1024, (h2 + 1) * 1024))
                S.op(POOL, lambda e, b=b: e.tensor_tensor(ctm.v("p (g i) -> p g i", i=128), CT4, cm3[:, b, :].unsqueeze(1).to_broadcast([128, 4, 128]), ALU.mult),
                     reads=xc.b(20 * T, 24 * T) + colmask.b(), writes=ctm.b())
                for g in range(4):
                    bk = PS()
                    mm(bk.f, [(ctm.ap[:, g * 128:(g + 1) * 128], h0T.ap[:, g * 512:(g + 1) * 512])], ctm.b() + h0T.b(), bk)
                    S.op(DVE, lambda e, g=g, bk=bk: e.tensor_tensor(yoff[g].ap, yoff[g].ap, bk.f, ALU.add), reads=bk.buf + yoff[g].b(), writes=yoff[g].b())
                S.op(POOL, lambda e, b=b: e.tensor_scalar_mul(bmk.ap, btk.ap, onehot.ap[:, b:b + 1]), reads=btk.b() + onehot.b(), writes=bmk.b())
                for q4 in range(4):
                    bk = PS()
                    for qq in range(4):
                        q = q4 * 4 + qq
                        gq = q // 4
                        mm(bk.f[:, qq * 128:(qq + 1) * 128], [(xdt.ap[:, q * 128:(q + 1) * 128], bmk.ap[:, gq * 128:(gq + 1) * 128])], xdt.b() + bmk.b(), bk)
                    for qq in range(4):
                        q = q4 * 4 + qq
                        S.op(DVE, lambda e, q=q, qq=qq, bk=bk, h0f=h0f, b=b: e.scalar_tensor_tensor(hout.ap[:, q * 128:(q + 1) * 128], h0f.ap[:, q * 128:(q + 1) * 128], d3[:, b, q:q + 1],
                                                                                      bk.f[:, qq * 128:(qq + 1) * 128], ALU.mult, ALU.add),
                             reads=bk.buf + h0f.b() + dcol.b(), writes=hout.b(q * 128, (q + 1) * 128))
                S.op(ACT, lambda e, b=b: e.dma_start(out=o_ssms[l, b].rearrange("(q w) p n -> (w p) q n", w=2), in_=hout.v("p (q n) -> p q n", n=128)), reads=hout.b(), dma_key="sth")
            for g in range(4):
                eac = ex.ap[:, g * 8:(g + 1) * 8].unsqueeze(2).to_broadcast([128, 8, 64])
                S.op(DVE, lambda e, g=g, eac=eac: e.tensor_tensor(yt.v("p (r d) -> p r d", d=64), yoff[g].v("p (r d) -> p r d", d=64), eac, ALU.mult), reads=yoff[g].b() + ex.b(), writes=yt.b())
                S.op(DVE, lambda e, g=g: e.tensor_tensor(yt.ap, yt.ap, ydg[g].ap, ALU.add), reads=ydg[g].b() + yt.b(), writes=yt.b())
                ssd_finish_group(l, P, g, 0, T, 0, xst, sz, yt, yb, xc, xc3, sm, ftmp)

        def ffn_layer(l, x_mid, x_dst):
            moe = (l % 2 == 1)
            xres = AR.alloc(NT * 1024, F32)
            x1T = AR.alloc(8 * NTOK, BF16)
            x1T3 = x1T.v("p (k t) -> p k t", t=NTOK)
            t1 = AR.alloc(512, F32); t2 = AR.alloc(512, F32)
            sm = AR.alloc(64, F32)
            wsc = AR.alloc(NT * 8, F32)
            wsc3 = wsc.v("p (t e) -> p t e", e=8)
            if moe:
                rw = AR.alloc(8 * 8, F32)
                load(SP, rw.b(), rw.v("p (k e) -> p k e", e=8), router_w[0].rearrange("(k p) e -> p k e", p=128), "ldp")
                rb = AR.alloc(8, F32)
                load(SP, rb.b(), rb.ap, router_b[0:1, :].partition_broadcast(128), "ldp")
                lg = AR.alloc(NT * 8, F32)
                lg3 = lg.v("p (t e) -> p t e", e=8)
                xtf = AR.alloc(1024, F32)
            for t in range(NT):
                xt_b = xres.b(t * 1024, (t + 1) * 1024)
                xt_ap = xres.ap[:, t * 1024:(t + 1) * 1024]
                load(SP, xt_b, xt_ap, x_mid[t * 128:(t + 1) * 128, :], f"ldf{t % 4}")
                for k4 in range(2):
                    bk = PS()
                    for kk in range(4):
                        k = k4 * 4 + kk
                        S.op(PE, lambda e, k=k, kk=kk, bk=bk, xt_ap=xt_ap: e.transpose(bk.f[:, kk * 128:(kk + 1) * 128], xt_ap[:, k * 128:(k + 1) * 128], ident.ap), reads=xt_b + ident.b(), writes=bk.buf)
                    dst = x1T3[:, k4 * 4:(k4 + 1) * 4, t * 128:(t + 1) * 128]
                    S.op(ACT, lambda e, dst=dst, bk=bk: e.copy(dst, bk.f.rearrange("p (k t) -> p k t", t=128)), reads=bk.buf, writes=x1T.b())
                    if moe:
                        S.op(DVE, lambda e, k4=k4, bk=bk: e.tensor_copy(xtf.ap[:, k4 * 512:(k4 + 1) * 512], bk.f), reads=bk.buf, writes=xtf.b(k4 * 512, (k4 + 1) * 512))
                if moe:
                    bk = PS()
                    mm(bk.f[:, 0:8], [(xtf.ap[:, k * 128:(k + 1) * 128], rw.ap[:, k * 8:(k + 1) * 8]) for k in range(8)], xtf.b() + rw.b(), bk)
                    S.op(DVE, lambda e, t=t, bk=bk: e.tensor_tensor(lg3[:, t, :], bk.f[:, 0:8], rb.ap, ALU.add), reads=bk.buf + rb.b(), writes=lg.b())
                S.op(POOL, lambda e, xt_ap=xt_ap: e.tensor_scalar_mul(xt_ap, xt_ap, ALPHA), reads=xt_b, writes=xt_b)
            ckpt(f'Fa{l}')
            if moe:
                m1 = AR.alloc(NT, F32); m2 = AR.alloc(NT, F32); mk1 = AR.alloc(NT * 8, F32); mk2 = AR.alloc(NT * 8, F32); l2_ = AR.alloc(NT * 8, F32)
                g1 = AR.alloc(NT, F32); g2 = AR.alloc(NT, F32)
                AX = mybir.AxisListType
                S.op(DVE, lambda e: e.reduce_max(m1.ap, lg3, AX.X), reads=lg.b(), writes=m1.b())
                S.op(DVE, lambda e: e.tensor_tensor(mk1.v("p (t e) -> p t e", e=8), lg3, m1.ap.unsqueeze(2).to_broadcast([128, NT, 8]), ALU.is_equal), reads=lg.b() + m1.b(), writes=mk1.b())
                S.op(DVE, lambda e: e.scalar_tensor_tensor(l2_.ap, mk1.ap, -1e30, lg.ap, ALU.mult, ALU.add), reads=mk1.b() + lg.b(), writes=l2_.b())
                S.op(DVE, lambda e: e.reduce_max(m2.ap, l2_.v("p (t e) -> p t e", e=8), AX.X), reads=l2_.b(), writes=m2.b())
                S.op(DVE, lambda e: e.tensor_tensor(mk2.v("p (t e) -> p t e", e=8), l2_.v("p (t e) -> p t e", e=8), m2.ap.unsqueeze(2).to_broadcast([128, NT, 8]), ALU.is_equal), reads=l2_.b() + m2.b(), writes=mk2.b())
                S.op(DVE, lambda e: e.tensor_sub(g2.ap, m2.ap, m1.ap), reads=m1.b() + m2.b(), writes=g2.b())
                S.op(ACT, lambda e: e.activation(g2.ap, g2.ap, AF.Exp), reads=g2.b(), writes=g2.b())
                S.op(DVE, lambda e: e.tensor_scalar_add(g1.ap, g2.ap, 1.0), reads=g2.b(), writes=g1.b())
                S.op(DVE, lambda e: e.reciprocal(g1.ap, g1.ap), reads=g1.b(), writes=g1.b())
                S.op(DVE, lambda e: e.tensor_tensor(g2.ap, g2.ap, g1.ap, ALU.mult), reads=g1.b() + g2.b(), writes=g2.b())
                S.op(DVE, lambda e: e.tensor_tensor(mk1.v("p (t e) -> p t e", e=8), mk1.v("p (t e) -> p t e", e=8), g1.ap.unsqueeze(2).to_broadcast([128, NT, 8]), ALU.mult), reads=mk1.b() + g1.b(), writes=mk1.b())
                S.op(DVE, lambda e: e.tensor_tensor(mk2.v("p (t e) -> p t e", e=8), mk2.v("p (t e) -> p t e", e=8), g2.ap.unsqueeze(2).to_broadcast([128, NT, 8]), ALU.mult), reads=mk2.b() + g2.b(), writes=mk2.b())
                S.op(DVE, lambda e: e.tensor_tensor(wsc.ap, mk1.ap, mk2.ap, ALU.add), reads=mk1.b() + mk2.b(), writes=wsc.b())
                S.op(DVE, lambda e: e.tensor_scalar_mul(wsc.ap, wsc.ap, 0.5), reads=wsc.b(), writes=wsc.b())
            else:
                memset(wsc, 0.5)

            ckpt(f'Fb{l}')
            hmark = AR.mark()
            hbuf = [AR.alloc(4 * NTOK, BF16), AR.alloc(4 * NTOK, BF16)]
            blocks = []
            if moe:
                for ex_ in range(NEXP):
                    for b7 in range(7):
                        blocks.append((moe_wg[0, ex_], moe_wu[0, ex_], moe_wd[0, ex_], b7 * 512, 4, ex_))
            else:
                for b6 in range(6):
                    blocks.append((ffn_wg[0], ffn_wu[0], ffn_wd[0], b6 * 512, 4 if b6 < 5 else 2, 0))
            sts = [(s * 512, 512) for s in range(4)] + [(2048, 128)]

            def phase1(bi):
                wg_, wu_, wd_, f0, nch, ex_ = blocks[bi]
                hb = hbuf[bi % 2]
                hb3 = hb.v("p (c t) -> p c t", t=NTOK)
                gw, gwb = wget(wv(wg_, 0, 8, f0, nch * 128), 8, nch * 128)
                uw, uwb = wget(wv(wu_, 0, 8, f0, nch * 128), 8, nch * 128)
                for c in range(nch):
                    for (s0, sn) in sts:
                        bg = PS()
                        mm(bg.f[:, 0:sn], [(gw[:, k, c * 128:(c + 1) * 128], x1T3[:, k, s0:s0 + sn]) for k in range(8)], gwb + x1T.b(), bg)
                        bu = PS()
                        mm(bu.f[:, 0:sn], [(uw[:, k, c * 128:(c + 1) * 128], x1T3[:, k, s0:s0 + sn]) for k in range(8)], uwb + x1T.b(), bu)
                        S.op(ACT, lambda e, bg=bg, sn=sn: e.activation(t1.ap[:, 0:sn], bg.f[:, 0:sn], AF.Tanh, scale=0.5), reads=bg.buf, writes=t1.b())
                        S.op(DVE, lambda e, bg=bg, sn=sn: e.scalar_tensor_tensor(t2.ap[:, 0:sn], t1.ap[:, 0:sn], 1.0, bg.f[:, 0:sn], ALU.add, ALU.mult), reads=bg.buf + t1.b(), writes=t2.b())
                        S.op(DVE, lambda e, bu=bu, sn=sn, s0=s0, c=c, hb3=hb3: e.tensor_tensor(hb3[:, c, s0:s0 + sn], t2.ap[:, 0:sn], bu.f[:, 0:sn], ALU.mult), reads=bu.buf + t2.b(), writes=hb.b(c * NTOK + s0, c * NTOK + s0 + sn))

            def phase2(bi):
                wg_, wu_, wd_, f0, nch, ex_ = blocks[bi]
                hb = hbuf[bi % 2]
                hb3 = hb.v("p (c t) -> p c t", t=NTOK)
                dw, dwb = wget(wd_[f0:f0 + nch * 128, :].rearrange("(c p) n -> p c n", p=128), nch, 1024)
                for t in range(NT):
                    for hf in range(2):
                        bk = PS()
                        mm(bk.f, [(hb3[:, c, t * 128:(t + 1) * 128], dw[:, c, hf * 512:(hf + 1) * 512]) for c in range(nch)], dwb + hb.b(), bk)
                        xa = xres.ap[:, t * 1024 + hf * 512: t * 1024 + (hf + 1) * 512]
                        xb = xres.b(t * 1024 + hf * 512, t * 1024 + (hf + 1) * 512)
                        S.op(DVE, lambda e, xa=xa, bk=bk, t=t, ex_=ex_: e.scalar_tensor_tensor(xa, bk.f, wsc3[:, t, ex_:ex_ + 1], xa, ALU.mult, ALU.add), reads=bk.buf + xb + wsc.b(), writes=xb)

            nb_ = len(blocks)
            phase1(0)
            for bi in range(nb_):
                if bi + 1 < nb_:
                    phase1(bi + 1)
                phase2(bi)
                ckpt(f'Fblk{l}')
            AR.release(hmark)
            l2g = AR.alloc(1024, F32); l2b = AR.alloc(1024, F32)
            load(SP, l2g.b(), l2g.ap, ln2_g[l:l + 1, :].partition_broadcast(128), "ldp")
            load(SP, l2b.b(), l2b.ap, ln2_b[l:l + 1, :].partition_broadcast(128), "ldp")
            for t in range(NT):
                xt_b = xres.b(t * 1024, (t + 1) * 1024)
                xt_ap = xres.ap[:, t * 1024:(t + 1) * 1024]
                layer_norm(xt_b, xt_ap, l2g, l2b, sm, EPS)
                S.op(ACT, lambda e, t=t, xt_ap=xt_ap: e.dma_start(out=x_dst[t * 128:(t + 1) * 128, :], in_=xt_ap), reads=xt_b, dma_key=f"sty{t % 4}")

        S.dry = True
        program()
        S.dry = False
        program()
        S.finalize(st)
        import os
        if os.environ.get('MK_DEBUG'):
            print('stats', S.stats, 'ops', {e: len(S.ops[e]) for e in ENGS}, 'arena hi', AR.hi, 'const top', CA.top, 'n_inst', nc.n_instructions() if callable(getattr(nc, 'n_instructions', None)) else None)
    return nc


_CACHE = {}


def kernel(**inp):
    f = lambda a: np.ascontiguousarray(np.asarray(a, dtype=np.float32))
    xp, xs = f(inp["x_prompt"]), f(inp["x_sample"])
    shared = {}
    for k in ("w_in", "conv_w", "dt_bias", "a_log", "d_skip", "v_ln_g", "v_ln_b", "w_s", "p_gm", "p_ssd", "p_xa", "w_out",
              "w_mem_k", "w_mem_v", "ln1_g", "ln1_b", "ln2_g", "ln2_b", "ffn_wg", "ffn_wu", "ffn_wd", "router_w", "router_b",
              "moe_wg", "moe_wu", "moe_wd"):
        shared[k] = f(inp[k])
    shared["conv_b"] = f(inp["conv_b"]).reshape(DEPTH, 1, 3072)
    shared["ssd_norm_g"] = f(inp["ssd_norm_g"]).reshape(DEPTH, 1, 2048)
    shared["b_s"] = f(inp["b_s"]).reshape(DEPTH, 1, 1024)
    memp = f(inp["mem_prompt"])
    ckk, cvv = f(inp["cache_mem_k"]), f(inp["cache_mem_v"])
    scv, ssm = f(inp["state_conv"]), f(inp["state_ssm"])
    in_maps = []
    for c in range(NCORES):
        m = dict(shared)
        m["xin"] = np.concatenate([xp[c], xs[16 * c:16 * (c + 1)].reshape(128, D)], axis=0)
        m["mem"] = memp[c]
        m["ck"] = np.ascontiguousarray(ckk[:, 16 * c:16 * (c + 1)].reshape(DEPTH, 16, 256, D))
        m["cv"] = np.ascontiguousarray(cvv[:, 16 * c:16 * (c + 1)].reshape(DEPTH, 16, 256, D))
        m["sconv"] = np.ascontiguousarray(scv[:, 16 * c:16 * (c + 1)].reshape(DEPTH, 48, 3072))
        m["sssm"] = np.ascontiguousarray(ssm[:, 16 * c:16 * (c + 1)])
        in_maps.append(m)
    if "nc" not in _CACHE:
        _CACHE["nc"] = build_program()
    res = run_bass_kernel_spmd(_CACHE["nc"], in_maps, core_ids=list(range(NCORES)))
    R = res.results
    y_prompt = np.stack([R[c]["o_y"][:2048] for c in range(NCORES)])
    y_sample = np.concatenate([R[c]["o_y"][2048:].reshape(16, 8, D) for c in range(NCORES)])
    memk = np.stack([R[c]["o_memk"] for c in range(NCORES)], axis=1).reshape(DEPTH, NCORES, 256, 4, 256)
    memv = np.stack([R[c]["o_memv"] for c in range(NCORES)], axis=1).reshape(DEPTH, NCORES, 256, 4, 256)
    convp = np.stack([R[c]["o_convp"] for c in range(NCORES)], axis=1)
    ssmp = np.stack([R[c]["o_ssmp"] for c in range(NCORES)], axis=1)
    convs = np.concatenate([R[c]["o_convs"].reshape(DEPTH, 16, 3, 3072) for c in range(NCORES)], axis=1)
    ssms = np.concatenate([R[c]["o_ssms"] for c in range(NCORES)], axis=1)
    gv = np.concatenate([R[c]["o_gv"].reshape(DEPTH, 16, 8, D) for c in range(NCORES)], axis=1)
    return (y_prompt.astype(np.float32), y_sample.astype(np.float32), memk.astype(np.float32), memv.astype(np.float32),
            convp.astype(np.float32), ssmp.astype(np.float32), convs.astype(np.float32), ssms.astype(np.float32), gv.astype(np.float32))
```
